# Optimizing a Trainium2 kernel written in Bass

```python
import math
import jax, jax.numpy as jnp
from jax import lax
import numpy as np

D_MODEL = 2048
BATCH = 2
SEQ = 8192
DEPTH = 4

N_META = 16
BLOCK = 128
META_PAD = BLOCK - N_META
EPS = 1e-6

HEAD_DIM = 128
MIX_WIDTH = D_MODEL
POOL_WIDTH = MIX_WIDTH // 4
ML_WIDTH = (MIX_WIDTH - POOL_WIDTH) // 2
SB_WIDTH = MIX_WIDTH - POOL_WIDTH - ML_WIDTH

ML_HEADS = ML_WIDTH // HEAD_DIM
ML_DV = HEAD_DIM
ML_DQK = HEAD_DIM // 2
ML_QK_WIDTH = ML_HEADS * ML_DQK
ML_CONV = 4

SB_HEADS = SB_WIDTH // HEAD_DIM
SB_DH = HEAD_DIM

POOL_WINDOWS = (2, 4, 8, 16)
POOL_GROUPS = len(POOL_WINDOWS)
POOL_CH = POOL_WIDTH // POOL_GROUPS

FFN_HIDDEN = -(-(8 * D_MODEL) // (3 * 256)) * 256

IN_SIZES = (ML_QK_WIDTH, ML_QK_WIDTH, ML_WIDTH, ML_WIDTH, ML_HEADS, ML_HEADS,
            SB_WIDTH, SB_WIDTH, SB_WIDTH, POOL_WIDTH)
IN_WIDTH = sum(IN_SIZES)

kernel_name = "hymba_mlstm_stickbreak_pool_hybrid"


def rmsnorm(x, g):
    xf = x.astype(jnp.float32)
    y = xf * lax.rsqrt(jnp.mean(xf * xf, axis=-1, keepdims=True) + EPS)
    return (y * g.astype(jnp.float32)).astype(x.dtype)


def split_cols(u, sizes):
    out, off = [], 0
    for s in sizes:
        out.append(u[..., off:off + s])
        off += s
    return out


def to_heads(x, n_heads):
    B, L, _ = x.shape
    return x.reshape(B, L, n_heads, -1).transpose(0, 2, 1, 3)


def from_heads(x):
    B, H, L, d = x.shape
    return x.transpose(0, 2, 1, 3).reshape(B, L, H * d)


def causal_dwconv(x, w):
    K, C = w.shape
    return lax.conv_general_dilated(
        x, w[:, None, :].astype(x.dtype), window_strides=(1,), padding=[(K - 1, 0)],
        dimension_numbers=('NWC', 'WIO', 'NWC'), feature_group_count=C)


def mlstm_chunkwise(q, k, v, log_i, log_f):
    f32 = jnp.float32
    B, H, L, dqk = q.shape
    dv = v.shape[-1]
    nc = L // BLOCK
    qc = q.astype(f32).reshape(B, H, nc, BLOCK, dqk)
    kc = (k.astype(f32) / math.sqrt(dqk)).reshape(B, H, nc, BLOCK, dqk)
    vc = v.astype(f32).reshape(B, H, nc, BLOCK, dv)
    ig = log_i.reshape(B, H, nc, BLOCK)
    b = jnp.cumsum(log_f.reshape(B, H, nc, BLOCK), axis=-1)
    b_last = b[..., -1]

    g = b_last[..., None] - b + ig
    m_loc = jnp.max(g, axis=-1)
    w_loc = jnp.exp(g - m_loc[..., None])
    C_loc = jnp.einsum('bhcsv,bhcsk->bhcvk', w_loc[..., None] * vc, kc)
    n_loc = jnp.einsum('bhcs,bhcsk->bhck', w_loc, kc)

    def step(carry, xs):
        C, n, m = carry
        Cl, nl, ml, bl = xs
        m_new = jnp.maximum(bl + m, ml)
        a = jnp.exp(bl + m - m_new)
        c = jnp.exp(ml - m_new)
        C_new = a[..., None, None] * C + c[..., None, None] * Cl
        n_new = a[..., None] * n + c[..., None] * nl
        return (C_new, n_new, m_new), (C, n, m)

    init = (jnp.zeros((B, H, dv, dqk), f32), jnp.zeros((B, H, dqk), f32), jnp.zeros((B, H), f32))
    xs = (jnp.moveaxis(C_loc, 2, 0), jnp.moveaxis(n_loc, 2, 0),
          jnp.moveaxis(m_loc, 2, 0), jnp.moveaxis(b_last, 2, 0))
    _, (C_prev, n_prev, m_prev) = lax.scan(step, init, xs)
    C_prev = jnp.moveaxis(C_prev, 0, 2)
    n_prev = jnp.moveaxis(n_prev, 0, 2)
    m_prev = jnp.moveaxis(m_prev, 0, 2)

    causal = jnp.tril(jnp.ones((BLOCK, BLOCK), bool))
    Dm = jnp.where(causal, b[..., :, None] - b[..., None, :] + ig[..., None, :], -jnp.inf)
    inter_log = b + m_prev[..., None]
    m_t = jnp.maximum(inter_log, jnp.max(Dm, axis=-1))
    S = jnp.einsum('bhctk,bhcsk->bhcts', qc, kc) * jnp.exp(Dm - m_t[..., None])
    a_inter = jnp.exp(inter_log - m_t)
    num = (jnp.einsum('bhcts,bhcsv->bhctv', S, vc)
           + a_inter[..., None] * jnp.einsum('bhcvk,bhctk->bhctv', C_prev, qc))
    den = jnp.sum(S, axis=-1) + a_inter * jnp.einsum('bhck,bhctk->bhct', n_prev, qc)
    hm = num / jnp.maximum(jnp.abs(den), jnp.exp(-m_t))[..., None]
    return hm.reshape(B, H, L, dv)


def stick_breaking(q, k, v, key_valid):
    f32 = jnp.float32
    B, H, L, d = q.shape
    nb = L // BLOCK
    scale = 1.0 / math.sqrt(d)
    kf = k.astype(f32)
    vf = v.astype(f32)
    kpos = jnp.arange(L)
    qb = jnp.moveaxis(q.astype(f32).reshape(B, H, nb, BLOCK, d), 2, 0)

    def block(args):
        q_blk, blk = args
        qpos = blk * BLOCK + jnp.arange(BLOCK)
        z = jnp.einsum('bhtd,bhsd->bhts', q_blk, kf) * scale
        live = (kpos[None, :] < qpos[:, None]) & key_valid[None, :]
        log_beta = jnp.where(live, jax.nn.log_sigmoid(z), -jnp.inf)
        log_1mb = jnp.where(live, jax.nn.log_sigmoid(-z), 0.0)
        later = lax.cumsum(log_1mb, axis=3, reverse=True) - log_1mb
        A = jnp.exp(log_beta + later)
        return jnp.einsum('bhts,bhsd->bhtd', A, vf)

    out = lax.map(block, (qb, jnp.arange(nb)))
    return jnp.moveaxis(out, 0, 2).reshape(B, H, L, d)


def multiscale_pool(u, valid, w_pool, pool_scale):
    f32 = jnp.float32
    B, L, _ = u.shape
    vf = valid.astype(f32)
    uf = u.astype(f32) * vf[None, :, None]
    maxw = max(POOL_WINDOWS)
    cs = jnp.pad(jnp.cumsum(uf, axis=1), ((0, 0), (maxw, 0), (0, 0)))
    cnt = jnp.pad(jnp.cumsum(vf), (maxw, 0))
    outs = []
    for g, w in enumerate(POOL_WINDOWS):
        lo, hi = g * POOL_CH, (g + 1) * POOL_CH
        s = cs[:, maxw:, lo:hi] - cs[:, maxw - w:maxw - w + L, lo:hi]
        c = cnt[maxw:] - cnt[maxw - w:maxw - w + L]
        outs.append(s / jnp.maximum(c, 1.0)[None, :, None] - uf[..., lo:hi])
    y = jnp.stack(outs, axis=2)
    y = jnp.einsum('blgc,gcd->blgd', y, w_pool.astype(f32)) * pool_scale.astype(f32)
    return y.reshape(B, L, POOL_WIDTH)


def token_mixing(h, valid, w_in, conv_w, ig_b, fg_b, pool_w, pool_scale, w_out, g_pre, g_post):
    f32 = jnp.float32
    xn = rmsnorm(h, g_pre)
    vmask = valid[None, :, None]
    u = jnp.einsum('bld,de->ble', xn, w_in) * vmask.astype(h.dtype)
    ml_q, ml_k, ml_v, ml_o, ml_i, ml_f, sb_q, sb_k, sb_v, pool_u = split_cols(u, IN_SIZES)

    qk = jax.nn.silu(causal_dwconv(jnp.concatenate([ml_q, ml_k], axis=-1), conv_w))
    ml_q, ml_k = qk[..., :ML_QK_WIDTH], qk[..., ML_QK_WIDTH:]
    log_i = jnp.where(vmask, ml_i.astype(f32) + ig_b.astype(f32), -jnp.inf)
    log_f = jnp.where(vmask, jax.nn.log_sigmoid(ml_f.astype(f32) + fg_b.astype(f32)), 0.0)
    hm = mlstm_chunkwise(to_heads(ml_q, ML_HEADS), to_heads(ml_k, ML_HEADS), to_heads(ml_v, ML_HEADS),
                         log_i.transpose(0, 2, 1), log_f.transpose(0, 2, 1))
    ml_out = from_heads(hm).astype(h.dtype) * jax.nn.sigmoid(ml_o)

    sb = stick_breaking(to_heads(sb_q, SB_HEADS), to_heads(sb_k, SB_HEADS), to_heads(sb_v, SB_HEADS), valid)
    sb_out = from_heads(sb).astype(h.dtype)

    pool_out = multiscale_pool(pool_u, valid, pool_w, pool_scale).astype(h.dtype)

    mix = jnp.concatenate([ml_out, sb_out, pool_out], axis=-1)
    y = jnp.einsum('ble,ed->bld', mix, w_out)
    return rmsnorm(y, g_post)


def channel_mixing(h, w_gate_up, w_down, g_pre, g_post):
    xn = rmsnorm(h, g_pre)
    gu = jnp.einsum('bld,df->blf', xn, w_gate_up)
    gate, up = gu[..., :FFN_HIDDEN], gu[..., FFN_HIDDEN:]
    y = jnp.einsum('blf,fd->bld', jax.nn.silu(gate) * up, w_down)
    return rmsnorm(y, g_post)


def setup_inputs(seed: int = 0) -> dict:
    key = jax.random.key(seed)
    ks = jax.random.split(key, 16)
    f32 = jnp.float32
    nrm = lambda k, shape, s: jax.random.normal(k, shape, f32) * s
    gain = lambda k: 1.0 + nrm(k, (DEPTH, D_MODEL), 0.02)
    fg_b = jnp.tile(jnp.linspace(3.0, 6.0, ML_HEADS, dtype=f32)[None, :], (DEPTH, 1)) + nrm(ks[5], (DEPTH, ML_HEADS), 0.1)
    return {
        'x': nrm(ks[0], (BATCH, SEQ, D_MODEL), 1.0),
        'meta_tokens': nrm(ks[1], (N_META, D_MODEL), 1.0),
        'w_in': nrm(ks[2], (DEPTH, D_MODEL, IN_WIDTH), D_MODEL ** -0.5),
        'ml_conv_w': nrm(ks[3], (DEPTH, ML_CONV, 2 * ML_QK_WIDTH), ML_CONV ** -0.5),
        'ml_igate_b': nrm(ks[4], (DEPTH, ML_HEADS), 0.1),
        'ml_fgate_b': fg_b,
        'pool_w': nrm(ks[6], (DEPTH, POOL_GROUPS, POOL_CH, POOL_CH), POOL_CH ** -0.5),
        'pool_scale': 1.0 + nrm(ks[7], (DEPTH, POOL_GROUPS, POOL_CH), 0.02),
        'w_out': nrm(ks[8], (DEPTH, MIX_WIDTH, D_MODEL), MIX_WIDTH ** -0.5),
        'g_mix_pre': gain(ks[9]),
        'g_mix_post': gain(ks[10]),
        'g_ffn_pre': gain(ks[11]),
        'g_ffn_post': gain(ks[12]),
        'w_gate_up': nrm(ks[13], (DEPTH, D_MODEL, 2 * FFN_HIDDEN), D_MODEL ** -0.5),
        'w_down': nrm(ks[14], (DEPTH, FFN_HIDDEN, D_MODEL), FFN_HIDDEN ** -0.5),
    }


def reference(x, meta_tokens, w_in, ml_conv_w, ml_igate_b, ml_fgate_b, pool_w, pool_scale, w_out,
              g_mix_pre, g_mix_post, g_ffn_pre, g_ffn_post, w_gate_up, w_down):
    B = x.shape[0]
    meta = jnp.broadcast_to(meta_tokens[None].astype(x.dtype), (B, N_META, D_MODEL))
    h = jnp.concatenate([jnp.zeros((B, META_PAD, D_MODEL), x.dtype), meta, x], axis=1)
    L = h.shape[1]
    valid = jnp.arange(L) >= META_PAD
    for l in range(DEPTH):
        h = h + token_mixing(h, valid, w_in[l], ml_conv_w[l], ml_igate_b[l], ml_fgate_b[l],
                             pool_w[l], pool_scale[l], w_out[l], g_mix_pre[l], g_mix_post[l])
        h = h + channel_mixing(h, w_gate_up[l], w_down[l], g_ffn_pre[l], g_ffn_post[l])
    return h[:, BLOCK:, :]
```

```python
import contextlib
import numpy as np
import concourse.bass as bass
import concourse.mybir as mybir
from concourse.bass_utils import run_bass_kernel_spmd

F32 = mybir.dt.float32
BF16 = mybir.dt.bfloat16
AF = mybir.ActivationFunctionType
ALU = mybir.AluOpType

ENGS = ("pe", "act", "dve", "pool", "sp")
NDMA = 6


class Sched:
    def __init__(self, nc, es):
        self.nc = nc
        self.es = es
        self.q = {e: [] for e in ENGS}
        self.sem = {}
        self.cnt = {}
        for e in ("pe", "act", "dve", "pool"):
            self.sem[e] = es.enter_context(nc.semaphore("s_" + e))
            self.cnt[e] = 0
        self.dring = {}
        for e in ("sp", "pool", "act"):
            ring = []
            for i in range(NDMA):
                nm = "d_%s%d" % (e, i)
                self.sem[nm] = es.enter_context(nc.semaphore(nm))
                self.cnt[nm] = 0
                ring.append(nm)
            self.dring[e] = [ring, 0]
        self.sem["cc"] = es.enter_context(nc.semaphore("s_cc"))
        self.cnt["cc"] = 0
        self.waited = {e: {} for e in ENGS}
        self.last_w = {}
        self.readers = {}
        self.nops = 0

    def _deps(self, reads, writes):
        deps = []
        for k in reads:
            t = self.last_w.get(k)
            if t is not None:
                deps.append(t)
        for k in writes:
            t = self.last_w.get(k)
            if t is not None:
                deps.append(t)
            deps.extend(self.readers.get(k, ()))
        return deps

    def _commit(self, tok, reads, writes):
        for k in reads:
            self.readers.setdefault(k, []).append(tok)
        for k in writes:
            self.last_w[k] = tok
            self.readers[k] = []

    def _waits(self, eng, deps, skip_self=False):
        best = {}
        for (s, v) in deps:
            if skip_self and s == eng:
                continue
            if best.get(s, 0) < v:
                best[s] = v
        out = []
        w = self.waited[eng]
        for s, v in best.items():
            if w.get(s, 0) < v:
                w[s] = v
                out.append((s, v))
        return out

    def op(self, eng, fn, reads=(), writes=(), deps=()):
        d = self._deps(reads, writes) + list(deps)
        waits = self._waits(eng, d, skip_self=(eng == "pe"))
        self.cnt[eng] += 1
        tok = (eng, self.cnt[eng])
        self.q[eng].append((waits, fn, (eng, 1)))
        self._commit(tok, reads, writes)
        self.nops += 1
        return tok

    def dma(self, fn, reads=(), writes=(), queue="sp", deps=()):
        ring, idx = self.dring[queue]
        nm = ring[idx % NDMA]
        self.dring[queue][1] = idx + 1
        d = self._deps(reads, writes) + list(deps)
        if self.cnt[nm] > 0:
            d.append((nm, self.cnt[nm]))
        waits = self._waits(queue, d)
        self.cnt[nm] += 16
        tok = (nm, self.cnt[nm])
        self.q[queue].append((waits, fn, (nm, 16)))
        self._commit(tok, reads, writes)
        self.nops += 1
        return tok

    def cc(self, fn, reads=(), writes=(), inc=1):
        d = self._deps(reads, writes)
        waits = self._waits("pool", d)
        self.cnt["cc"] += inc
        tok = ("cc", self.cnt["cc"])
        self.q["pool"].append((waits, fn, ("cc", inc)))
        self._commit(tok, reads, writes)
        return tok

    def barrier(self):
        allt = [(s, v) for s, v in self.cnt.items() if v > 0]
        for e in ENGS:
            waits = self._waits(e, allt)
            if waits:
                self.q[e].append((waits, None, None))
        self.last_w = {}
        self.readers = {}

    def emit(self, final_tokens=()):
        nc = self.nc
        waits = self._waits("sp", list(final_tokens))
        if waits:
            self.q["sp"].append((waits, None, None))
        sem = self.sem
        with nc.Block() as block:
            def runner(name):
                def run(eng):
                    for (waits, fn, inc) in self.q[name]:
                        for (s, v) in waits:
                            eng.wait_ge(sem[s], v)
                        if fn is not None:
                            fn(eng).then_inc(sem[inc[0]], inc[1])
                return run
            block.tensor(runner("pe"))
            block.scalar(runner("act"))
            block.vector(runner("dve"))
            block.gpsimd(runner("pool"))
            block.sync(runner("sp"))
        self.q = {e: [] for e in ENGS}


D = 2048
TC = 17 * 128
NK = 16
EPS = 1e-6
TT = [(0, 512), (512, 512), (1024, 512), (1536, 512), (2048, 128)]
IN_W = 5132
FF = 5632
NKEY = 16 + 8192
QSCALE = float(1.0 / np.sqrt(128.0))
LN8 = float(np.log(8.0))
GROUPS = [[0, 1, 2, 3], [4, 5, 6, 7]]
FM_GROUPS = [
    ("qkpre", 0, 384, 0), ("qkpre", 384, 384, 384),
    ("mlo", 1536, 384, 0), ("mlo", 1920, 384, 384),
    ("gates", 2304, 12, 0),
    ("sbq", 2316, 384, 0), ("sbq", 2700, 384, 384),
    ("sbk", 3084, 384, 0), ("sbk", 3468, 384, 384),
    ("poolu", 4620, 512, 0),
]
TM_GROUPS = [("mlv", 768, 384, 0), ("mlv", 1152, 384, 3), ("sbv", 3852, 384, 0), ("sbv", 4236, 384, 3)]


class Ctx:
    uid = [0]

    def __init__(self, nc, S):
        self.nc = nc
        self.S = S
        self.es = contextlib.ExitStack()
        Ctx.uid[0] += 1
        self.pfx = "p%d_" % Ctx.uid[0]

    def sb(self, name, shape, dt):
        return self.es.enter_context(self.nc.sbuf_tensor(self.pfx + name, list(shape), dt))

    def ps(self, name, shape=(128, 512), dt=F32):
        return self.es.enter_context(self.nc.psum_tensor(self.pfx + name, list(shape), dt))

    def close(self):
        self.S.barrier()
        self.S.emit()
        self.es.close()


def emit_rstd(C, src, srck, sq, sqk, rr, ones_bf, bank, bankk, tn):
    S = C.S
    S.op("act", lambda e: e.activation(sq[:, :, 0:tn], src[:, :, 0:tn], AF.Square), reads=[srck], writes=[sqk])
    for k in range(NK):
        S.op("pe", lambda e, k=k: e.matmul(bank[:, 0:tn], ones_bf[:], sq[:, k, 0:tn], start=(k == 0), stop=(k == NK - 1)), reads=[sqk, "ones"], writes=[bankk])
    S.op("act", lambda e: e.activation(rr[:, 0:tn], bank[:, 0:tn], AF.Sqrt, bias=EPS, scale=1.0 / D), reads=[bankk], writes=["rr"])
    S.op("dve", lambda e: e.reciprocal(rr[:, 0:tn], rr[:, 0:tn]), reads=["rr"], writes=["rr"])


def phase_p1(nc, S, hT, w_in_l, g_pre_l, X):
    C = Ctx(nc, S)
    banks = [C.ps("bank%d" % i) for i in range(8)]
    ones_bf = C.sb("ones", [128, 128], BF16)
    gs = C.sb("gs", [128, NK], F32)
    xn = C.sb("xn", [128, NK, TC], BF16)
    ht = C.sb("rn_h", [128, NK, 512], F32)
    sq = C.sb("rn_sq", [128, NK, 512], BF16)
    rr = C.sb("rn_r", [128, 512], F32)
    S.op("pool", lambda e: e.memset(ones_bf[:], 1.0), writes=["ones"])
    S.dma(lambda e: e.dma_start(out=gs[:], in_=g_pre_l), writes=["gs"])
    hv = hT.rearrange("(k p) t -> p k t", p=128)

    def norm_tile(t0, tn):
        S.dma(lambda e: e.dma_start(out=ht[:, :, 0:tn], in_=hv[:, :, t0:t0 + tn]), writes=["rn_h"])
        emit_rstd(C, ht, "rn_h", sq, "rn_sq", rr, ones_bf, banks[7], "bank7", tn)
        for k in range(NK):
            S.op("dve", lambda e, k=k: e.scalar_tensor_tensor(xn[:, k, t0:t0 + tn], ht[:, k, 0:tn], gs[:, k:k + 1], rr[:, 0:tn], ALU.mult, ALU.mult),
                 reads=["rn_h", "rr", "gs"], writes=["xn%d" % (t0 // 512)])
        if t0 == 0:
            S.op("pool", lambda e: e.memset(xn[:, :, 0:112], 0.0), writes=["xn0"])
    for (t0, tn) in TT:
        norm_tile(t0, tn)
    xkeys = ["xn%d" % i for i in range(5)]
    wv = w_in_l.rearrange("(k p) e -> p k e", p=128)
    wb = [C.sb("wb%d" % i, [128, NK, 512], BF16) for i in range(2)]
    odt = {"qkpre": F32, "mlo": BF16, "gates": F32, "sbq": BF16, "sbk": BF16, "poolu": F32}
    stg = {F32: [C.sb("stf%d" % i, [128, 512], F32) for i in range(3)], BF16: [C.sb("stb%d" % i, [128, 512], BF16) for i in range(3)]}
    sti = {F32: 0, BF16: 0}
    gi = 0
    bi = 0
    for (name, c0, ncol, r0) in FM_GROUPS:
        w = wb[gi % 2]
        wk = "wb%d" % (gi % 2)
        gi += 1
        S.dma(lambda e, w=w, c0=c0, ncol=ncol: e.dma_start(out=w[:, :, 0:ncol], in_=wv[:, :, c0:c0 + ncol]), writes=[wk], queue="pool")
        dt = odt[name]
        for ti, (t0, tn) in enumerate(TT):
            for j0 in range(0, ncol, 128):
                jn = min(128, ncol - j0)
                bk = bi % 6
                bi += 1
                pb = banks[bk]
                for k in range(NK):
                    S.op("pe", lambda e, pb=pb, w=w, k=k, j0=j0, jn=jn, t0=t0, tn=tn: e.matmul(
                        pb[0:jn, 0:tn], w[:, k, j0:j0 + jn], xn[:, k, t0:t0 + tn], start=(k == 0), stop=(k == NK - 1)),
                        reads=[wk, xkeys[ti]], writes=["bank%d" % bk])
                si = sti[dt] % 3
                sti[dt] += 1
                st = stg[dt][si]
                sk = "st%s%d" % ("f" if dt == F32 else "b", si)
                if name == "mlo":
                    S.op("act", lambda e, st=st, pb=pb, jn=jn, tn=tn: e.activation(st[0:jn, 0:tn], pb[0:jn, 0:tn], AF.Sigmoid), reads=["bank%d" % bk], writes=[sk])
                else:
                    S.op("dve", lambda e, st=st, pb=pb, jn=jn, tn=tn: e.tensor_copy(st[0:jn, 0:tn], pb[0:jn, 0:tn]), reads=["bank%d" % bk], writes=[sk])
                o = X[name]
                S.dma(lambda e, o=o, st=st, r=r0 + j0, jn=jn, t0=t0, tn=tn: e.dma_start(out=o[r:r + jn, t0:t0 + tn], in_=st[0:jn, 0:tn]), reads=[sk], writes=["o_" + name])
    for (name, c0, ncol, h0) in TM_GROUPS:
        w = wb[gi % 2]
        wk = "wb%d" % (gi % 2)
        gi += 1
        S.dma(lambda e, w=w, c0=c0, ncol=ncol: e.dma_start(out=w[:, :, 0:ncol], in_=wv[:, :, c0:c0 + ncol]), writes=[wk], queue="pool")
        for b in range(17):
            bk = bi % 6
            bi += 1
            pb = banks[bk]
            for k in range(NK):
                S.op("pe", lambda e, pb=pb, w=w, k=k, b=b, ncol=ncol: e.matmul(
                    pb[:, 0:ncol], xn[:, k, b * 128:(b + 1) * 128], w[:, k, 0:ncol], start=(k == 0), stop=(k == NK - 1)),
                    reads=[wk, xkeys[min(b // 4, 4)]], writes=["bank%d" % bk])
            si = sti[BF16] % 3
            sti[BF16] += 1
            st = stg[BF16][si]
            sk = "stb%d" % si
            S.op("dve", lambda e, st=st, pb=pb, ncol=ncol: e.tensor_copy(st[:, 0:ncol], pb[:, 0:ncol]), reads=["bank%d" % bk], writes=[sk])
            o = X[name]
            for hh in range(3):
                S.dma(lambda e, o=o, st=st, b=b, hh=hh, h0=h0: e.dma_start(out=o[h0 + hh, b * 128:(b + 1) * 128, :], in_=st[:, hh * 128:(hh + 1) * 128]),
                      reads=[sk], writes=["o_" + name])
    C.close()
    C = Ctx(nc, S)
    tl = C.sb("tl", [128, 82], F32)
    S.dma(lambda e: e.dma_start(out=tl[:, 0:18].rearrange("p (c j) -> p c j", j=3), in_=X["qkpre"].rearrange("(c p) t -> p c t", p=128)[:, :, TC - 3:TC]), writes=["tl"])
    S.dma(lambda e: e.dma_start(out=tl[:, 18:82].rearrange("p (g j) -> p g j", j=16), in_=X["poolu"].rearrange("(g p) t -> p g t", p=128)[:, :, TC - 16:TC]), writes=["tl"])
    S.dma(lambda e: e.dma_start(out=X["tail"][:, :], in_=tl[:]), reads=["tl"], writes=["tail"])
    C.close()


def phase_gather1(nc, S, X):
    S.cc(lambda e: e.collective_compute("AllGather", ALU.bypass, replica_groups=GROUPS, ins=[X["tail"][:, :].opt()], outs=[X["gT"][:, :].opt()]), writes=["gT"])
    for h in range(6):
        S.cc(lambda e, h=h: e.collective_compute("AllGather", ALU.bypass, replica_groups=GROUPS, ins=[X["sbk"][h * 128:(h + 1) * 128, :].opt()], outs=[X["gK"][h].opt()]), writes=["gK%d" % h])
        S.cc(lambda e, h=h: e.collective_compute("AllGather", ALU.bypass, replica_groups=GROUPS, ins=[X["sbq"][h * 128:(h + 1) * 128, :].opt()], outs=[X["gQ"][h].opt()]), writes=["gQ%d" % h])
        S.cc(lambda e, h=h: e.collective_compute("AllGather", ALU.bypass, replica_groups=GROUPS, ins=[X["sbv"][h].opt()], outs=[X["gV"][h].opt()]), writes=["gV%d" % h])


def phase_attn(nc, S, X, Cst):
    C = Ctx(nc, S)
    sbo = X["sbo_c"]
    zb = [C.ps("zb%d" % i) for i in range(2)]
    ab = [C.ps("ab%d" % i) for i in range(2)]
    cb = [C.ps("cb%d" % i) for i in range(2)]
    ob = [C.ps("ob%d" % i) for i in range(2)]
    m4 = C.sb("m4", [128, 4, 128], BF16)
    mm = C.sb("mm", [16, 128], BF16)
    idt = C.sb("idt", [128, 128], BF16)
    ntr = C.sb("ntr", [128, 128], BF16)
    onec = C.sb("onec", [128, 1], BF16)
    negr = C.sb("negr", [1, 128], BF16)
    sq4 = C.sb("sq4", [128, 4], F32)
    S.dma(lambda e: e.dma_start(out=m4[:], in_=Cst["mask4"][:, :, :]), writes=["consts"])
    S.dma(lambda e: e.dma_start(out=mm[:], in_=Cst["maskm"][:, :]), writes=["consts"])
    S.dma(lambda e: e.dma_start(out=idt[:], in_=Cst["identb"][:, :]), writes=["consts"])
    S.dma(lambda e: e.dma_start(out=ntr[:], in_=Cst["ntri"][:, :]), writes=["consts"])
    S.dma(lambda e: e.dma_start(out=sq4[:], in_=Cst["selq4"][:, :]), writes=["sq4"])
    S.op("pool", lambda e: e.memset(onec[:], 1.0), writes=["consts2"])
    S.op("pool", lambda e: e.memset(negr[:], -1.0), writes=["consts2"])
    Kh = [C.sb("Kh%d" % i, [128, NKEY], BF16) for i in range(2)]
    Vh = [C.sb("Vh%d" % i, [128, 64, 128], BF16) for i in range(2)]
    Vm = [C.sb("Vm%d" % i, [16, 128], BF16) for i in range(2)]
    Qh = [C.sb("Qh%d" % i, [128, TC], BF16) for i in range(2)]
    Qa = C.sb("Qa", [128, 8192], BF16)
    ee = [C.sb("ee%d" % i, [128, 512], F32) for i in range(2)]
    sp = [C.sb("sp%d" % i, [128, 512], BF16) for i in range(2)]
    AA = [C.sb("AA%d" % i, [128, 512], BF16) for i in range(2)]
    cf = [[C.sb("cf%d_%d" % (i, j), [1, 5, 128], F32) for j in range(2)] for i in range(2)]
    crow = [C.sb("crow%d" % i, [1, 4, 128], BF16) for i in range(2)]
    osb = [C.sb("osb%d" % i, [128, 128], BF16) for i in range(2)]
    cfp = [C.sb("cfp%d" % i, [1, 128], F32) for i in range(2)]
    cpb = [C.sb("cpb%d" % i, [1, 128], BF16) for i in range(2)]
    nones = C.sb("nones", [128, 128], BF16)
    S.op("pool", lambda e: e.memset(nones[:], -1.0), writes=["consts2"])

    class Stream:
        pass

    def run_slot_pair(hb, h, slots):
        K, V, VM, Q = Kh[hb], Vh[hb], Vm[hb], Qh[hb]
        kk = ["Kh%d" % hb, "Vh%d" % hb, "Qh%d" % hb]
        sts = []
        for (si, a) in slots:
            st = Stream()
            st.si = si
            st.a = a
            st.qc = 0 if a < 0 else (1 + a) * 128
            if a < 0:
                st.groups = [("metaq", None)]
            else:
                st.groups = [("mask", [4 * a + 3, 4 * a + 2, 4 * a + 1, 4 * a])]
                for g in range(a - 1, -1, -1):
                    st.groups.append(("live", [4 * g + 3, 4 * g + 2, 4 * g + 1, 4 * g]))
                st.groups.append(("meta", None))
            st.cfi = 0
            st.first_av = True
            sts.append(st)
        ng = max(len(st.groups) for st in sts)

        def qk(st, dst, dkey, kind, blocks, start_first):
            q = Q[:, st.qc:st.qc + 128]
            if kind in ("meta", "metaq"):
                S.op("pe", lambda e: e.matmul(dst[0:16, 0:128], K[:, 0:16], q, start=True, stop=(kind == "meta")), reads=kk, writes=[dkey])
                if kind == "metaq":
                    S.op("pe", lambda e: e.matmul(dst[0:16, 0:128], idt[0:16, 0:16], mm[:, :], start=False, stop=True), reads=["consts"], writes=[dkey])
                return
            for n, blk in enumerate(blocks):
                kc = 16 + blk * 128
                S.op("pe", lambda e, n=n, kc=kc: e.matmul(dst[:, n * 128:(n + 1) * 128], K[:, kc:kc + 128], q, start=True, stop=(kind != "mask")),
                     reads=kk, writes=[dkey])
                if kind == "mask":
                    jj = 3 - n
                    S.op("pe", lambda e, n=n, jj=jj: e.matmul(dst[:, n * 128:(n + 1) * 128], idt[:], m4[:, jj, :], start=False, stop=True),
                         reads=["consts"], writes=[dkey])

        def stage1(st, gi):
            kind, blocks = st.groups[gi]
            qk(st, zb[st.si], "zb%d" % st.si, kind, blocks, True)

        def stage2(st, gi):
            kind, blocks = st.groups[gi]
            si = st.si
            z = zb[si]
            np_, nf = (16, 128) if kind in ("meta", "metaq") else (128, 512)
            S.op("act", lambda e: e.activation(ee[si][0:np_, 0:nf], z[0:np_, 0:nf], AF.Exp), reads=["zb%d" % si], writes=["ee%d" % si])
            S.op("act", lambda e: e.activation(sp[si][0:np_, 0:nf], ee[si][0:np_, 0:nf], AF.Ln, bias=1.0, scale=1.0), reads=["ee%d" % si], writes=["sp%d" % si])

        def stage3(st, gi):
            kind, blocks = st.groups[gi]
            si = st.si
            a_ = ab[si]
            ak = "ab%d" % si
            q = Q[:, st.qc:st.qc + 128]
            ck = "cpb%d" % si
            if kind in ("meta", "metaq"):
                S.op("pe", lambda e: e.matmul(a_[0:16, 0:128], ntr[0:16, 0:16], sp[si][0:16, 0:128], start=True, stop=False), reads=["sp%d" % si, "consts"], writes=[ak])
                S.op("pe", lambda e: e.matmul(a_[0:16, 0:128], K[:, 0:16], q, start=False, stop=False), reads=kk, writes=[ak])
                if kind == "metaq":
                    S.op("pe", lambda e: e.matmul(a_[0:16, 0:128], idt[0:16, 0:16], mm[:, :], start=False, stop=True), reads=["consts"], writes=[ak])
                else:
                    S.op("pe", lambda e: e.matmul(a_[0:16, 0:128], negr[0:1, 0:16], cpb[si][0:1, :], start=False, stop=True), reads=[ck, "consts2"], writes=[ak])
                return
            S.op("pe", lambda e: e.matmul(a_[:, 0:512], ntr[:], sp[si][:, 0:512], start=True, stop=False), reads=["sp%d" % si, "consts"], writes=[ak])
            for n, blk in enumerate(blocks):
                kc = 16 + blk * 128
                dst = a_[:, n * 128:(n + 1) * 128]
                S.op("pe", lambda e, kc=kc, dst=dst: e.matmul(dst, K[:, kc:kc + 128], q, start=False, stop=False), reads=kk, writes=[ak])
                if kind == "mask":
                    S.op("pe", lambda e, dst=dst, jj=3 - n: e.matmul(dst, idt[:], m4[:, jj, :], start=False, stop=False), reads=["consts"], writes=[ak])
            for m in range(3):
                src = sp[si][:, m * 128:(m + 1) * 128]
                rep = bass.AP(src.tensor, src.offset, [list(src.ap[0]), [0, 3 - m], list(src.ap[1])])
                dstv = a_[:, (m + 1) * 128:512].rearrange("p (r t) -> p r t", t=128)
                S.op("pe", lambda e, rep=rep, dstv=dstv: e.matmul(dstv, nones[:], rep, start=False, stop=False), reads=["sp%d" % si, "consts2"], writes=[ak])
            crep = bass.AP(cpb[si][0:1, :].tensor, cpb[si][0:1, :].offset, [list(cpb[si][0:1, :].ap[0]), [0, 4], list(cpb[si][0:1, :].ap[1])])
            S.op("pe", lambda e, crep=crep: e.matmul(a_[:, 0:512].rearrange("p (r t) -> p r t", t=128), negr[0:1, 0:128], crep, start=False, stop=True),
                 reads=[ck, "consts2"], writes=[ak])
            for n in range(4):
                S.op("pe", lambda e, n=n: e.matmul(cb[si][0:1, 0:128], onec[:, 0:1], sp[si][:, n * 128:(n + 1) * 128], start=(n == 0), stop=(n == 3)),
                     reads=["sp%d" % si, "consts2"], writes=["cb%d" % si])
            S.op("dve", lambda e: e.tensor_tensor(cfp[si][0:1, :], cfp[si][0:1, :], cb[si][0:1, 0:128], ALU.add), reads=["cfp%d" % si, "cb%d" % si], writes=["cfp%d" % si])
            S.op("dve", lambda e: e.tensor_copy(cpb[si][0:1, :], cfp[si][0:1, :]), reads=["cfp%d" % si], writes=[ck])

        def stage4(st, gi):
            kind, blocks = st.groups[gi]
            si = st.si
            np_, nf = (16, 128) if kind in ("meta", "metaq") else (128, 512)
            S.op("act", lambda e: e.activation(AA[si][0:np_, 0:nf], ab[si][0:np_, 0:nf], AF.Exp), reads=["ab%d" % si], writes=["AA%d" % si])

        def stage5(st, gi):
            kind, blocks = st.groups[gi]
            si = st.si
            o = ob[si][:, 0:128]
            last = (gi == len(st.groups) - 1)
            if kind in ("meta", "metaq"):
                S.op("pe", lambda e, fa=st.first_av: e.matmul(o, VM[0:16, :], AA[si][0:16, 0:128], start=fa, stop=True), reads=["AA%d" % si] + kk, writes=["ob%d" % si])
                st.first_av = False
            else:
                for n, blk in enumerate(blocks):
                    S.op("pe", lambda e, n=n, blk=blk, fa=st.first_av: e.matmul(o, V[:, blk, :], AA[si][:, n * 128:(n + 1) * 128], start=fa, stop=False),
                         reads=["AA%d" % si] + kk, writes=["ob%d" % si])
                    st.first_av = False
            if last:
                S.op("dve", lambda e: e.tensor_copy(osb[si][:], o), reads=["ob%d" % si], writes=["osb%d" % si])
                S.dma(lambda e, qc=st.qc: e.dma_start(out=sbo[h * 128:(h + 1) * 128, qc:qc + 128], in_=osb[si][:]), reads=["osb%d" % si], writes=["sbo"])

        for st in sts:
            si = st.si
            S.op("dve", lambda e, si=si: e.memset(cfp[si][0:1, :], 0.0), writes=["cfp%d" % si])
            S.op("dve", lambda e, si=si: e.memset(cpb[si][0:1, :], 0.0), writes=["cpb%d" % si])
            stage1(st, 0)
        for gi in range(ng):
            act = [st for st in sts if gi < len(st.groups)]
            for st in act:
                stage2(st, gi)
            for st in sts:
                if gi + 1 < len(st.groups):
                    stage1(st, gi + 1)
            for st in act:
                stage3(st, gi)
            for st in act:
                stage4(st, gi)
            for st in act:
                stage5(st, gi)

    for h in range(6):
        hb = h % 2
        K_, V_, Q_ = Kh[hb], Vh[hb], Qh[hb]
        S.dma(lambda e, K_=K_, h=h: e.dma_start(out=K_[:, 0:16], in_=X["sbk"][h * 128:(h + 1) * 128, 112:128]), writes=["Kh%d" % hb])
        S.dma(lambda e, hb=hb, h=h: e.dma_start(out=Vm[hb][:], in_=X["sbv"][h, 112:128, :]), writes=["Vh%d" % hb])
        for r in range(4):
            S.dma(lambda e, K_=K_, h=h, r=r: e.dma_start(out=K_[:, 16 + r * 2048:16 + (r + 1) * 2048], in_=X["gK"][h, r * 128:(r + 1) * 128, 128:TC]), reads=["gK%d" % h], writes=["Kh%d" % hb])
            S.dma(lambda e, V_=V_, h=h, r=r: e.dma_start(out=V_[:, r * 16:(r + 1) * 16, :], in_=X["gV"][h, r * TC + 128:(r + 1) * TC, :].rearrange("(n p) e -> p n e", p=128)),
                  reads=["gV%d" % h], writes=["Vh%d" % hb])
            S.dma(lambda e, h=h, r=r: e.dma_start(out=Qa[:, r * 2048:(r + 1) * 2048], in_=X["gQ"][h, r * 128:(r + 1) * 128, 128:TC]), reads=["gQ%d" % h], writes=["Qa"])
        S.dma(lambda e, Q_=Q_, h=h: e.dma_start(out=Q_[:, 0:128], in_=X["sbq"][h * 128:(h + 1) * 128, 0:128]), writes=["Qh%d" % hb])
        S.op("pool", lambda e, Q_=Q_: e.tensor_scalar(Q_[:, 0:128], Q_[:, 0:128], QSCALE, None, ALU.mult), reads=["Qh%d" % hb], writes=["Qh%d" % hb])
        qsel = Q_[:, 128:TC].rearrange("p (a t) -> p a t", t=128)
        qall = Qa[:].rearrange("p (a j t) -> p a j t", j=4, t=128)
        S.op("dve", lambda e, qsel=qsel, qall=qall: e.tensor_scalar(qsel, qall[:, :, 0, :], sq4[:, 0:1], None, ALU.mult), reads=["Qa", "sq4"], writes=["Qh%d" % hb])
        for jj in range(1, 4):
            S.op("dve", lambda e, qsel=qsel, qall=qall, jj=jj: e.scalar_tensor_tensor(qsel, qall[:, :, jj, :], sq4[:, jj:jj + 1], qsel, ALU.mult, ALU.add),
                 reads=["Qa", "sq4", "Qh%d" % hb], writes=["Qh%d" % hb])
        run_slot_pair(hb, h, [(0, -1)])
        for m in range(8):
            run_slot_pair(hb, h, [(0, 15 - m), (1, m)])
    C.close()


def phase_ml(nc, S, X, Cst, convw_l, gbias_l, pool_w_l, pscale_l):
    C = Ctx(nc, S)
    pg = C.ps("pg"); pbrow = C.ps("pbrow"); pT = C.ps("pT", (128, 512), BF16); pS = C.ps("pS"); pN = C.ps("pN"); pD = C.ps("pD")
    pE = C.ps("pE"); pC = C.ps("pC")
    triu = C.sb("triu", [128, 128], F32); ones = C.sb("ones", [128, 128], F32); i6 = C.sb("i6", [6, 6], F32); selh = C.sb("selh", [6, 6, 128], F32)
    mask01 = C.sb("mask01", [128, 128], F32); identb = C.sb("identb", [128, 128], BF16)
    cw = C.sb("cw", [128, 6, 4], F32); gb = C.sb("gb", [6, 2], F32); ngb = C.sb("ngb", [6, 1], F32)
    selp = C.sb("selp", [128, 5], F32); selq = C.sb("selq", [128, 4], F32)
    for (t, d, k) in [(triu, Cst["triu"], "triu"), (ones, Cst["ones128"], "ones"), (i6, Cst["i6"], "i6"), (mask01, Cst["mask01"], "mask01"),
                      (identb, Cst["identb"], "identb"), (selp, Cst["selprev"], "selp"), (selq, Cst["selq"], "selq")]:
        S.dma(lambda e, t=t, d=d: e.dma_start(out=t[:], in_=d[:, :]), writes=[k])
    S.dma(lambda e: e.dma_start(out=gb[:], in_=gbias_l), writes=["gb"])
    S.dma(lambda e: e.dma_start(out=selh[:], in_=Cst["selh"][:, :, :]), writes=["selh"])
    S.dma(lambda e: e.dma_start(out=cw[:], in_=convw_l), writes=["cw"])
    S.op("dve", lambda e: e.tensor_scalar(ngb[:], gb[:, 1:2], -1.0, None, ALU.mult), reads=["gb"], writes=["ngb"])

    gT = C.sb("gT", [128, 4, 82], F32); mt = C.sb("mt", [128, 82], F32); hb_ = C.sb("hb", [128, 82], F32)
    S.dma(lambda e: e.dma_start(out=gT[:], in_=X["gT"].rearrange("(r p) x -> p r x", p=128)), reads=["gT"], writes=["sgT"])
    S.dma(lambda e: e.dma_start(out=mt[:, 0:18].rearrange("p (c j) -> p c j", j=3), in_=X["qkpre"].rearrange("(c p) t -> p c t", p=128)[:, :, 125:128]), writes=["mt"])
    S.dma(lambda e: e.dma_start(out=mt[:, 18:82].rearrange("p (g j) -> p g j", j=16), in_=X["poolu"].rearrange("(g p) t -> p g t", p=128)[:, :, 112:128]), writes=["mt"])
    S.op("dve", lambda e: e.tensor_scalar(hb_[:], mt[:], selp[:, 4:5], None, ALU.mult), reads=["mt", "selp"], writes=["hb"])
    for r in range(4):
        S.op("dve", lambda e, r=r: e.scalar_tensor_tensor(hb_[:], gT[:, r, :], selp[:, r:r + 1], hb_[:], ALU.mult, ALU.add), reads=["sgT", "selp", "hb"], writes=["hb"])

    S.dma(lambda e: e.dma_start(out=X["hsel"][:, :], in_=hb_[:]), reads=["hb"], writes=["hsel"])

    qk = C.sb("qk", [128, 6, TC], BF16)
    XW = 3 + 128 + 3 + 2048
    xb = [C.sb("xb0", [128, XW], F32)] * 2
    yb = [C.sb("yb0", [128, TC], F32)] * 2
    for c in range(6):
        x = xb[0]; y = yb[0]; xk = "xb0"; yk = "yb0"
        S.op("pool", lambda e, x=x: e.memset(x[:, 0:3], 0.0), writes=[xk])
        S.dma(lambda e, x=x, c=c: e.dma_start(out=x[:, 3:131], in_=X["qkpre"][c * 128:(c + 1) * 128, 0:128]), writes=[xk])
        S.op("pool", lambda e, x=x, c=c: e.tensor_copy(x[:, 131:134], hb_[:, c * 3:(c + 1) * 3]), reads=["hb"], writes=[xk])
        S.dma(lambda e, x=x, c=c: e.dma_start(out=x[:, 134:XW], in_=X["qkpre"][c * 128:(c + 1) * 128, 128:TC]), writes=[xk])
        for (x0, y0, n) in [(0, 0, 128), (131, 128, 2048)]:
            S.op("dve", lambda e, x=x, y=y, c=c, x0=x0, y0=y0, n=n: e.tensor_scalar(y[:, y0:y0 + n], x[:, x0 + 3:x0 + 3 + n], cw[:, c, 3:4], None, ALU.mult),
                 reads=[xk, "cw"], writes=[yk])
            for j in range(3):
                S.op("dve", lambda e, x=x, y=y, c=c, x0=x0, y0=y0, n=n, j=j: e.scalar_tensor_tensor(
                    y[:, y0:y0 + n], x[:, x0 + j:x0 + j + n], cw[:, c, j:j + 1], y[:, y0:y0 + n], ALU.mult, ALU.add), reads=[xk, "cw", yk], writes=[yk])
        S.op("act", lambda e, y=y, c=c: e.activation(qk[:, c, :], y[:], AF.Silu), reads=[yk], writes=["qk"])

    gi_ = C.sb("gi", [6, TC], F32); gf_ = C.sb("gf", [6, TC], F32)
    S.dma(lambda e: e.dma_start(out=gi_[:], in_=X["gates"][0:6, :]), writes=["gi"])
    S.dma(lambda e: e.dma_start(out=gf_[:], in_=X["gates"][6:12, :]), writes=["gf"])
    S.op("act", lambda e: e.activation(gi_[:], gi_[:], AF.Identity, bias=gb[:, 0:1], scale=1.0), reads=["gi", "gb"], writes=["gi"])
    S.op("pool", lambda e: e.memset(gi_[:, 0:112], -30000.0), reads=["gi"], writes=["gi"])
    S.op("act", lambda e: e.activation(gf_[:], gf_[:], AF.Exp, bias=ngb[:, 0:1], scale=-1.0), reads=["gf", "ngb"], writes=["gf"])
    S.op("act", lambda e: e.activation(gf_[:], gf_[:], AF.Ln, bias=1.0, scale=1.0), reads=["gf"], writes=["gf"])
    S.op("pool", lambda e: e.memset(gf_[:, 0:112], 0.0), reads=["gf"], writes=["gf"])

    Va = C.sb("Va", [128, 17, 6, 256], BF16)
    S.op("pool", lambda e: e.memset(Va[:, :, :, 128:256], 1.0), writes=["Va1"])
    for h in range(6):
        S.dma(lambda e, h=h: e.dma_start(out=Va[:, :, h, 0:128], in_=X["mlv"][h].rearrange("(n p) v -> p n v", p=128)), writes=["Va0"])
    so = C.sb("so", [128, 6, TC], BF16)
    S.dma(lambda e: e.dma_start(out=so[:], in_=X["mlo"].rearrange("(h p) t -> p h t", p=128)), writes=["so"])

    Sst = C.sb("Sst", [128, 3, 256], F32); Sbf = C.sb("Sbf", [128, 3, 256], BF16); Sin = C.sb("Sin", [128, 3, 256], F32)
    Mst = C.sb("Mst", [128, 3, 256], F32); blob = C.sb("blob", [128, 772], F32); G = C.sb("G", [128, 4, 772], F32)
    lfT = C.sb("lfT", [128, 6], F32); iT = C.sb("iT", [128, 6], F32); tmp6 = C.sb("tmp6", [128, 6], F32); wcol = C.sb("wcol", [128, 6], F32)
    EcolA = C.sb("EcolA", [128, 17, 6], F32); eblA = C.sb("eblA", [128, 17, 6], F32); ebrA = C.sb("ebrA", [6, 17, 128], F32)
    wkA = C.sb("wkA", [128, 17, 3, 128], BF16)
    Sw = C.sb("Sw", [128, 128], BF16)
    aden = C.sb("aden", [128, 128], F32); hm = C.sb("hm", [128, 128], F32)
    mo = [C.sb("mo%d" % i, [128, 128], BF16) for i in range(2)]
    moi = [0]

    def chunk_gates(c):
        tsl = slice(c * 128, (c + 1) * 128)
        Ecol = EcolA[:, c, :]; ebl = eblA[:, c, :]
        S.op("pe", lambda e: e.matmul(pg[:, 0:6], gf_[:, tsl], i6[:], start=True, stop=True), reads=["gf", "i6"], writes=["pg"])
        S.op("pe", lambda e: e.matmul(pg[:, 6:12], gi_[:, tsl], i6[:], start=True, stop=True), reads=["gi", "i6"], writes=["pg"])
        S.op("dve", lambda e: e.tensor_copy(lfT[:], pg[:, 0:6]), reads=["pg"], writes=["lfT"])
        S.op("dve", lambda e: e.tensor_copy(iT[:], pg[:, 6:12]), reads=["pg"], writes=["iT"])
        S.op("pe", lambda e: e.matmul(pg[:, 12:18], triu[:], lfT[:], start=True, stop=True), reads=["lfT", "triu"], writes=["pg"])
        S.op("pe", lambda e: e.matmul(pg[:, 18:24], ones[:], lfT[:], start=True, stop=True), reads=["lfT", "ones"], writes=["pg"])
        S.op("pe", lambda e: e.matmul(pbrow[0:6, 0:128], lfT[:], triu[:], start=True, stop=True), reads=["lfT", "triu"], writes=["pbrow"])
        S.op("dve", lambda e: e.tensor_tensor(tmp6[:], iT[:], pg[:, 12:18], ALU.add), reads=["iT", "pg"], writes=["tmp6"])
        S.op("act", lambda e: e.activation(Ecol, tmp6[:], AF.Exp, bias=-LN8, scale=1.0), reads=["tmp6"], writes=["Ecol"])
        S.op("act", lambda e: e.activation(ebl, pg[:, 18:24], AF.Exp, scale=-1.0), reads=["pg"], writes=["ebl"])
        S.op("dve", lambda e: e.tensor_tensor(wcol[:], Ecol, ebl, ALU.mult), reads=["Ecol", "ebl"], writes=["wcol"])
        S.op("act", lambda e: e.activation(ebrA[:, c, :], pbrow[0:6, 0:128], AF.Exp), reads=["pbrow"], writes=["ebr"])
        for p in range(3):
            S.op("pe", lambda e, p=p: e.transpose(pT[:, p * 128:(p + 1) * 128], qk[:, 3 + p, tsl], identb[:]), reads=["qk", "identb"], writes=["pT"])
        for h in range(6):
            S.op("dve", lambda e, h=h: e.tensor_scalar(wkA[:, c, h // 2, (h % 2) * 64:(h % 2) * 64 + 64], pT[:, h * 64:(h + 1) * 64], wcol[:, h:h + 1], None, ALU.mult),
                 reads=["pT", "wcol"], writes=["wk"])

    def state_update(c, h):
        p = h // 2
        rs = slice((h % 2) * 64, (h % 2) * 64 + 64)
        S.op("pe", lambda e: e.matmul(pC[:, 0:256], wkA[:, c, p, :], Va[:, c, h, :], start=True, stop=True), reads=["wk", "Va0", "Va1"], writes=["pC"])
        S.op("dve", lambda e: e.scalar_tensor_tensor(Sst[rs, p, :], Sst[rs, p, :], eblA[rs, c, h:h + 1], pC[rs, 0:256], ALU.mult, ALU.add),
             reads=["Sst", "ebl", "pC", "Sbf"], writes=["Sst"])

    def outputs(c, h):
        tsl = slice(c * 128, (c + 1) * 128)
        p = h // 2
        rs = slice((h % 2) * 64, (h % 2) * 64 + 64)
        S.op("pe", lambda e: e.matmul(pS[:, 0:128], qk[rs, 3 + p, tsl], qk[rs, p, tsl], start=True, stop=True), reads=["qk"], writes=["pS"])
        S.op("dve", lambda e: e.scalar_tensor_tensor(Sw[:], pS[:, 0:128], EcolA[:, c, h:h + 1], mask01[:], ALU.mult, ALU.mult), reads=["pS", "Ecol", "mask01"], writes=["Sw"])
        S.op("pe", lambda e: e.matmul(pN[:, 0:128], Va[:, c, h, 0:128], Sw[:], start=True, stop=False), reads=["Va0", "Sw"], writes=["pN"])
        S.op("pe", lambda e: e.matmul(pN[:, 0:128], Sbf[rs, p, 0:128], qk[rs, p, tsl], start=False, stop=True), reads=["Sbf", "qk"], writes=["pN"])
        S.op("pe", lambda e: e.matmul(pD[:, 0:128], Va[:, c, h, 128:256], Sw[:], start=True, stop=False), reads=["Va1", "Sw"], writes=["pD"])
        S.op("pe", lambda e: e.matmul(pD[:, 0:128], Sbf[rs, p, 128:256], qk[rs, p, tsl], start=False, stop=True), reads=["Sbf", "qk"], writes=["pD"])
        S.op("pe", lambda e: e.matmul(pE[:, 0:128], selh[:, h, :], ebrA[:, c, :], start=True, stop=True), reads=["selh", "ebr"], writes=["pE"])
        S.op("act", lambda e: e.activation(aden[:], pD[:, 0:128], AF.Abs), reads=["pD"], writes=["aden"])
        S.op("dve", lambda e: e.tensor_tensor(aden[:], aden[:], pE[:, 0:128], ALU.max), reads=["aden", "pE"], writes=["aden"])
        S.op("dve", lambda e: e.reciprocal(aden[:], aden[:]), reads=["aden"], writes=["aden"])
        S.op("dve", lambda e: e.tensor_tensor(hm[:], pN[:, 0:128], aden[:], ALU.mult), reads=["pN", "aden"], writes=["hm"])
        m_ = mo[moi[0] % 2]; mk = "mo%d" % (moi[0] % 2); moi[0] += 1
        S.op("pool", lambda e: e.tensor_tensor(m_[:], hm[:], so[:, h, tsl], ALU.mult), reads=["hm", "so"], writes=[mk])
        S.dma(lambda e: e.dma_start(out=X["mlout"][h * 128:(h + 1) * 128, tsl], in_=m_[:]), reads=[mk], writes=["mlout"])

    S.op("pool", lambda e: e.memset(Sst[:], 0.0), writes=["Sst"])
    S.op("pool", lambda e: e.memset(blob[:, 768:772], 1.0), writes=["blobP"])
    for c in range(17):
        chunk_gates(c)
        if c == 1:
            S.op("act", lambda e: e.activation(Mst[:], Sst[:], AF.Copy), reads=["Sst"], writes=["Mst"])
            S.op("pool", lambda e: e.memset(Sst[:], 0.0), reads=["Mst"], writes=["Sst"])
        for h in range(6):
            state_update(c, h)
            if c >= 1:
                p = h // 2
                rs = slice((h % 2) * 64, (h % 2) * 64 + 64)
                S.op("pool", lambda e, p=p, rs=rs, h=h, c=c: e.tensor_tensor(blob[rs, 768 + p:769 + p], blob[rs, 768 + p:769 + p], eblA[rs, c, h:h + 1], ALU.mult),
                     reads=["ebl", "blobP"], writes=["blobP"])
    S.op("act", lambda e: e.activation(blob[:, 0:768], Sst[:].rearrange("p a b -> p (a b)"), AF.Copy), reads=["Sst"], writes=["blobS"])
    S.dma(lambda e: e.dma_start(out=X["stblob"][:, :], in_=blob[:]), reads=["blobS", "blobP"], writes=["stblob"])
    S.cc(lambda e: e.collective_compute("AllGather", ALU.bypass, replica_groups=GROUPS, ins=[X["stblob"][:, :].opt()], outs=[X["gS"][:, :].opt()]),
         reads=["stblob"], writes=["gS"])
    S.dma(lambda e: e.dma_start(out=G[:], in_=X["gS"].rearrange("(r p) x -> p r x", p=128)), reads=["gS"], writes=["G"])
    S.op("dve", lambda e: e.tensor_scalar(Sin[:], Mst[:], selq[:, 0:1], None, ALU.mult), reads=["Mst", "selq"], writes=["Sin"])
    for r in range(3):
        for p in range(3):
            S.op("dve", lambda e, r=r, p=p: e.scalar_tensor_tensor(Mst[:, p, :], Mst[:, p, :], G[:, r, 768 + p:769 + p], G[:, r, p * 256:(p + 1) * 256], ALU.mult, ALU.add),
                 reads=["Mst", "G"], writes=["Mst"])
        S.op("dve", lambda e, r=r: e.scalar_tensor_tensor(Sin[:], Mst[:], selq[:, r + 1:r + 2], Sin[:], ALU.mult, ALU.add), reads=["Mst", "selq", "Sin"], writes=["Sin"])
    S.op("pool", lambda e: e.memset(Sst[:], 0.0), reads=["blobS"], writes=["Sst"])
    S.op("pool", lambda e: e.memset(Sbf[:], 0.0), writes=["Sbf"])
    for c in range(17):
        if c == 1:
            S.op("act", lambda e: e.activation(Sst[:], Sin[:], AF.Copy), reads=["Sin", "Sst"], writes=["Sst"])
            S.op("act", lambda e: e.activation(Sbf[:], Sst[:], AF.Copy), reads=["Sst"], writes=["Sbf"])
        for h in range(6):
            outputs(c, h)
            state_update(c, h)
        S.op("act", lambda e: e.activation(Sbf[:], Sst[:], AF.Copy), reads=["Sst"], writes=["Sbf"])

    C.close()

    C = Ctx(nc, S)
    pC = C.ps("pC")
    hb_ = C.sb("hb", [128, 82], F32)
    S.dma(lambda e: e.dma_start(out=hb_[:], in_=X["hsel"][:, :]), writes=["hb"])
    PW = 16 + 128 + 16 + 2048
    px = C.sb("px", [128, PW], F32); pa = C.sb("pa", [128, PW], F32); pb_ = C.sb("pb", [128, PW], F32)
    py = C.sb("py", [128, TC], BF16); pwb = C.sb("pwb", [128, 4, 128], BF16); psc = C.sb("psc", [128, 4], F32); icn = C.sb("icn", [128, 4, 128], F32)
    S.dma(lambda e: e.dma_start(out=pwb[:], in_=pool_w_l.rearrange("g c d -> c g d")), writes=["pwb"], queue="pool")
    S.dma(lambda e: e.dma_start(out=psc[:], in_=pscale_l), writes=["psc"])
    S.dma(lambda e: e.dma_start(out=icn[:], in_=Cst["invcnt"][:, :, :]), writes=["icn"])
    post = [C.sb("post%d" % i, [128, 512], BF16) for i in range(2)]
    pi = 0
    for g in range(4):
        gs_ = slice(g * 128, (g + 1) * 128)
        S.op("pool", lambda e: e.memset(px[:, 0:16], 0.0), writes=["px"])
        S.dma(lambda e, gs_=gs_: e.dma_start(out=px[:, 16:144], in_=X["poolu"][gs_, 0:128]), writes=["px"])
        S.op("pool", lambda e, g=g: e.tensor_copy(px[:, 144:160], hb_[:, 18 + g * 16:18 + (g + 1) * 16]), reads=["hb"], writes=["px"])
        S.dma(lambda e, gs_=gs_: e.dma_start(out=px[:, 160:PW], in_=X["poolu"][gs_, 128:TC]), writes=["px"])
        S.op("pool", lambda e: e.tensor_tensor(pa[:, 1:PW], px[:, 1:PW], px[:, 0:PW - 1], ALU.add), reads=["px"], writes=["pa"])
        src, sk = pa, "pa"
        if g >= 1:
            S.op("pool", lambda e: e.tensor_tensor(pb_[:, 3:PW], pa[:, 3:PW], pa[:, 1:PW - 2], ALU.add), reads=["pa"], writes=["pb"])
            src, sk = pb_, "pb"
        if g >= 2:
            S.op("pool", lambda e: e.tensor_tensor(pa[:, 7:PW], pb_[:, 7:PW], pb_[:, 3:PW - 4], ALU.add), reads=["pb", "pa"], writes=["pa"])
            src, sk = pa, "pa"
        if g >= 3:
            S.op("pool", lambda e: e.tensor_tensor(pb_[:, 15:PW], pa[:, 15:PW], pa[:, 7:PW - 8], ALU.add), reads=["pa", "pb"], writes=["pb"])
            src, sk = pb_, "pb"
        w = float(2 ** (g + 1))
        S.op("dve", lambda e, g=g, src=src: e.tensor_tensor(src[:, 16:144], src[:, 16:144], icn[:, g, :], ALU.mult), reads=[sk, "icn"], writes=[sk])
        S.op("dve", lambda e, src=src: e.tensor_tensor(py[:, 0:128], src[:, 16:144], px[:, 16:144], ALU.subtract), reads=[sk, "px"], writes=["py"])
        S.op("dve", lambda e, src=src, w=w: e.scalar_tensor_tensor(py[:, 128:TC], src[:, 160:PW], 1.0 / w, px[:, 160:PW], ALU.mult, ALU.subtract),
             reads=[sk, "px"], writes=["py"])
        for (t0, tn) in TT:
            S.op("pe", lambda e, g=g, t0=t0, tn=tn: e.matmul(pC[:, 0:tn], pwb[:, g, :], py[:, t0:t0 + tn], start=True, stop=True), reads=["pwb", "py"], writes=["pC"])
            po = post[pi % 2]; pk = "post%d" % (pi % 2); pi += 1
            S.op("act", lambda e, po=po, g=g, tn=tn: e.activation(po[:, 0:tn], pC[:, 0:tn], AF.Copy, scale=psc[:, g:g + 1]), reads=["pC", "psc"], writes=[pk])
            S.dma(lambda e, po=po, g=g, t0=t0, tn=tn: e.dma_start(out=X["poolout"][g * 128:(g + 1) * 128, t0:t0 + tn], in_=po[:, 0:tn]), reads=[pk], writes=["poolout"])
    C.close()


def phase_gather2(nc, S, X):
    for h in range(6):
        S.cc(lambda e, h=h: e.collective_compute("AllGather", ALU.bypass, replica_groups=GROUPS, ins=[X["sbo_c"][h * 128:(h + 1) * 128, :].opt()], outs=[X["gO"][h].opt()]),
             writes=["gO"])


def phase_unshuffle(nc, S, X, Cst):
    C = Ctx(nc, S)
    selq = C.sb("selq", [128, 4], F32)
    S.dma(lambda e: e.dma_start(out=selq[:], in_=Cst["selq"][:, :]), writes=["selq"])
    cd = [[C.sb("cd%d_%d" % (i, qq), [128, 16, 128], BF16) for qq in range(4)] for i in range(2)]
    acc = [C.sb("acc%d" % i, [128, 16, 128], BF16) for i in range(2)]
    S.dma(lambda e: e.dma_start(out=X["sbo_n"][:, 0:128], in_=X["sbo_c"][:, 0:128]), writes=["sbo_n0"])
    for h in range(6):
        i = h % 2
        for qq in range(4):
            dst = cd[i][qq][:].rearrange("p (jh jl) t -> p jh jl t", jl=4)
            for jl in range(4):
                S.dma(lambda e, dst=dst, h=h, qq=qq, jl=jl: e.dma_start(
                    out=dst[:, :, jl, :], in_=X["gO"][h, jl * 128:(jl + 1) * 128, (1 + 4 * qq) * 128:(5 + 4 * qq) * 128].rearrange("p (jh t) -> p jh t", t=128)),
                    writes=["cd%d_%d" % (i, qq)])
        a_ = acc[i]
        S.op("dve", lambda e, a_=a_, i=i: e.tensor_scalar(a_[:], cd[i][0][:], selq[:, 0:1], None, ALU.mult), reads=["cd%d_0" % i, "selq"], writes=["acc%d" % i])
        for qq in range(1, 4):
            S.op("dve", lambda e, a_=a_, i=i, qq=qq: e.scalar_tensor_tensor(a_[:], cd[i][qq][:], selq[:, qq:qq + 1], a_[:], ALU.mult, ALU.add),
                 reads=["cd%d_%d" % (i, qq), "selq", "acc%d" % i], writes=["acc%d" % i])
        S.dma(lambda e, a_=a_, h=h: e.dma_start(out=X["sbo_n"][h * 128:(h + 1) * 128, 128:TC], in_=a_[:].rearrange("p j t -> p (j t)")), reads=["acc%d" % i], writes=["sbo_n"])
    C.close()


def phase_p3(nc, S, X, hT, hN, w_out_l, gains_l, w_gu_l, w_dn_l):
    C = Ctx(nc, S)
    banks = [C.ps("bank%d" % i) for i in range(8)]
    ones_bf = C.sb("ones", [128, 128], BF16)
    S.op("pool", lambda e: e.memset(ones_bf[:], 1.0), writes=["ones"])
    gn = C.sb("gn", [128, 3, NK], F32)
    S.dma(lambda e: e.dma_start(out=gn[:], in_=gains_l), writes=["gn"])
    B1 = C.sb("B1", [128, NK, 512], BF16); B2 = C.sb("B2", [128, NK, 512], BF16)
    Y = C.sb("Y", [128, NK, 512], F32); H = C.sb("H", [128, NK, 512], F32)
    rr = C.sb("rr", [128, 512], F32); sl = C.sb("sl", [128, 512], F32)
    hid = C.sb("hid", [128, 22, 512], BF16)
    wo = [C.sb("wo%d" % i, [128, NK, 256], BF16) for i in range(2)]
    wg = [C.sb("wg%d" % i, [128, NK, 256], BF16) for i in range(2)]
    wu = [C.sb("wu%d" % i, [128, NK, 256], BF16) for i in range(2)]
    wd = [C.sb("wd%d" % i, [128, 22, 256], BF16) for i in range(2)]
    hv = hT.rearrange("(k p) t -> p k t", p=128)
    hnv = hN.rearrange("(k p) t -> p k t", p=128)
    wov = w_out_l.rearrange("(k p) d -> p k d", p=128)
    wgv = w_gu_l.rearrange("(k p) f -> p k f", p=128)
    wdv = w_dn_l.rearrange("(f p) d -> p f d", p=128)
    cnt = {"wo": 0, "wg": 0, "wd": 0, "bk": 0}

    def rstd(src, srck, sq, sqk, tn):
        emit_rstd(C, src, srck, sq, sqk, rr, ones_bf, banks[7], "bank7", tn)

    def do_tile(t0, tn):
        S.dma(lambda e: e.dma_start(out=H[:, :, 0:tn], in_=hv[:, :, t0:t0 + tn]), writes=["H"])
        S.dma(lambda e: e.dma_start(out=B1[:, 0:6, 0:tn], in_=X["mlout"].rearrange("(k p) t -> p k t", p=128)[:, :, t0:t0 + tn]), writes=["B1"])
        S.dma(lambda e: e.dma_start(out=B1[:, 6:12, 0:tn], in_=X["sbo_n"].rearrange("(k p) t -> p k t", p=128)[:, :, t0:t0 + tn]), writes=["B1"])
        S.dma(lambda e: e.dma_start(out=B1[:, 12:16, 0:tn], in_=X["poolout"].rearrange("(k p) t -> p k t", p=128)[:, :, t0:t0 + tn]), writes=["B1"])
        for dch in range(NK):
            if dch % 2 == 0:
                w = wo[cnt["wo"] % 2]; wk_ = "wo%d" % (cnt["wo"] % 2); cnt["wo"] += 1
                S.dma(lambda e, w=w, dch=dch: e.dma_start(out=w[:], in_=wov[:, :, dch * 128:(dch + 2) * 128]), writes=[wk_], queue="pool")
            bk = cnt["bk"] % 4; cnt["bk"] += 1
            for k in range(NK):
                S.op("pe", lambda e, w=w, k=k, bk=bk, c0=(dch % 2) * 128: e.matmul(banks[bk][:, 0:tn], w[:, k, c0:c0 + 128], B1[:, k, 0:tn], start=(k == 0), stop=(k == NK - 1)),
                     reads=[wk_, "B1"], writes=["bank%d" % bk])
            S.op("dve", lambda e, dch=dch, bk=bk: e.tensor_copy(Y[:, dch, 0:tn], banks[bk][:, 0:tn]), reads=["bank%d" % bk], writes=["Y"])
        rstd(Y, "Y", B2, "B2", tn)
        for k in range(NK):
            S.op("dve", lambda e, k=k: e.scalar_tensor_tensor(Y[:, k, 0:tn], Y[:, k, 0:tn], gn[:, 0, k:k + 1], rr[:, 0:tn], ALU.mult, ALU.mult), reads=["Y", "gn", "rr"], writes=["Y"])
        S.op("pool", lambda e: e.tensor_tensor(H[:, :, 0:tn], H[:, :, 0:tn], Y[:, :, 0:tn], ALU.add), reads=["H", "Y"], writes=["H"])
        rstd(H, "H", B1, "B1", tn)
        for k in range(NK):
            S.op("dve", lambda e, k=k: e.scalar_tensor_tensor(B2[:, k, 0:tn], H[:, k, 0:tn], gn[:, 1, k:k + 1], rr[:, 0:tn], ALU.mult, ALU.mult), reads=["H", "gn", "rr"], writes=["B2"])
        for half in range(2):
            for fp in range(11):
                f0 = half * 22 + fp * 2
                g_ = wg[cnt["wg"] % 2]; u_ = wu[cnt["wg"] % 2]; gk = "wg%d" % (cnt["wg"] % 2); uk = "wu%d" % (cnt["wg"] % 2); cnt["wg"] += 1
                S.dma(lambda e, g_=g_, f0=f0: e.dma_start(out=g_[:], in_=wgv[:, :, f0 * 128:f0 * 128 + 256]), writes=[gk], queue="pool")
                S.dma(lambda e, u_=u_, f0=f0: e.dma_start(out=u_[:], in_=wgv[:, :, FF + f0 * 128:FF + f0 * 128 + 256]), writes=[uk], queue="pool")
                for j in range(2):
                    bg = (cnt["bk"] % 2); cnt["bk"] += 1
                    pg_, pu_ = banks[bg], banks[2 + bg]
                    for k in range(NK):
                        S.op("pe", lambda e, g_=g_, k=k, j=j, pg_=pg_: e.matmul(pg_[:, 0:tn], g_[:, k, j * 128:(j + 1) * 128], B2[:, k, 0:tn], start=(k == 0), stop=(k == NK - 1)),
                             reads=[gk, "B2"], writes=["bank%d" % bg])
                    for k in range(NK):
                        S.op("pe", lambda e, u_=u_, k=k, j=j, pu_=pu_: e.matmul(pu_[:, 0:tn], u_[:, k, j * 128:(j + 1) * 128], B2[:, k, 0:tn], start=(k == 0), stop=(k == NK - 1)),
                             reads=[uk, "B2"], writes=["bank%d" % (2 + bg)])
                    S.op("act", lambda e, pg_=pg_: e.activation(sl[:, 0:tn], pg_[:, 0:tn], AF.Silu), reads=["bank%d" % bg], writes=["sl"])
                    S.op("dve", lambda e, pu_=pu_, fi=fp * 2 + j: e.tensor_tensor(hid[:, fi, 0:tn], sl[:, 0:tn], pu_[:, 0:tn], ALU.mult),
                         reads=["sl", "bank%d" % (2 + bg)], writes=["hid"])
            for dch in range(NK):
                if dch % 2 == 0:
                    w = wd[cnt["wd"] % 2]; wk_ = "wd%d" % (cnt["wd"] % 2); cnt["wd"] += 1
                    S.dma(lambda e, w=w, dch=dch, half=half: e.dma_start(out=w[:], in_=wdv[:, half * 22:(half + 1) * 22, dch * 128:(dch + 2) * 128]), writes=[wk_], queue="pool")
                bk = 4 + (cnt["bk"] % 3); cnt["bk"] += 1
                for fi in range(22):
                    S.op("pe", lambda e, w=w, fi=fi, bk=bk, c0=(dch % 2) * 128: e.matmul(banks[bk][:, 0:tn], w[:, fi, c0:c0 + 128], hid[:, fi, 0:tn], start=(fi == 0), stop=(fi == 21)),
                         reads=[wk_, "hid"], writes=["bank%d" % bk])
                if half == 0:
                    S.op("dve", lambda e, dch=dch, bk=bk: e.tensor_copy(Y[:, dch, 0:tn], banks[bk][:, 0:tn]), reads=["bank%d" % bk], writes=["Y"])
                else:
                    S.op("dve", lambda e, dch=dch, bk=bk: e.tensor_tensor(Y[:, dch, 0:tn], Y[:, dch, 0:tn], banks[bk][:, 0:tn], ALU.add), reads=["bank%d" % bk, "Y"], writes=["Y"])
        rstd(Y, "Y", B1, "B1", tn)
        for k in range(NK):
            S.op("dve", lambda e, k=k: e.scalar_tensor_tensor(Y[:, k, 0:tn], Y[:, k, 0:tn], gn[:, 2, k:k + 1], rr[:, 0:tn], ALU.mult, ALU.mult), reads=["Y", "gn", "rr"], writes=["Y"])
        S.op("pool", lambda e: e.tensor_tensor(H[:, :, 0:tn], H[:, :, 0:tn], Y[:, :, 0:tn], ALU.add), reads=["H", "Y"], writes=["H"])
        return S.dma(lambda e: e.dma_start(out=hnv[:, :, t0:t0 + tn], in_=H[:, :, 0:tn]), reads=["H"], writes=["hN"])
    toks = []
    for (t0_, tn_) in TT:
        toks.append(do_tile(t0_, tn_))
    C.close()
    return toks


def build_fused(NL):
    nc = bass.Bass("TRN2", target_bir_lowering=False)
    def din(name, shape, dt=F32):
        return nc.dram_tensor(name, list(shape), dt, kind="ExternalInput").ap()
    def dint(name, shape, dt=F32):
        return nc.dram_tensor(name, list(shape), dt).ap()
    hT0 = din("hT0", [D, TC])
    w_in = din("w_in", [NL, D, IN_W]); g_pre = din("g_pre", [NL, 128, NK]); convw = din("convw", [NL, 128, 6, 4]); gbias = din("gbias", [NL, 6, 2])
    pool_w = din("pool_w", [NL, 4, 128, 128]); pscale = din("pscale", [NL, 128, 4]); w_out = din("w_out", [NL, D, D]); gains = din("gains", [NL, 128, 3, NK])
    w_gu = din("w_gu", [NL, D, 2 * FF]); w_dn = din("w_dn", [NL, FF, D])
    Cst = {"mask4": din("mask4", [128, 4, 128], BF16), "maskm": din("maskm", [16, 128], BF16), "identb": din("identb", [128, 128], BF16),
           "ntri": din("ntri", [128, 128], BF16), "selq4": din("selq4", [128, 4]), "selq": din("selq", [128, 4]), "selprev": din("selprev", [128, 5]),
           "triu": din("triu", [128, 128]), "ones128": din("ones128", [128, 128]), "i6": din("i6", [6, 6]), "selh": din("selh", [6, 6, 128]),
           "mask01": din("mask01", [128, 128]), "invcnt": din("invcnt", [128, 4, 128])}
    hN = nc.dram_tensor("hN", [D, TC], F32, kind="ExternalOutput").ap()
    X = {"qkpre": dint("x_qkpre", [768, TC]), "mlo": dint("x_mlo", [768, TC], BF16), "gates": dint("x_gates", [12, TC]),
         "sbq": dint("x_sbq", [768, TC], BF16), "sbk": dint("x_sbk", [768, TC], BF16), "poolu": dint("x_poolu", [512, TC]),
         "mlv": dint("x_mlv", [6, TC, 128], BF16), "sbv": dint("x_sbv", [6, TC, 128], BF16), "tail": dint("x_tail", [128, 82]),
         "gK": dint("x_gK", [6, 512, TC], BF16), "gQ": dint("x_gQ", [6, 512, TC], BF16), "gV": dint("x_gV", [6, 4 * TC, 128], BF16), "gT": dint("x_gT", [512, 82]),
         "sbo_c": dint("x_sbo_c", [768, TC], BF16), "gO": dint("x_gO", [6, 512, TC], BF16), "sbo_n": dint("x_sbo_n", [768, TC], BF16),
         "mlout": dint("x_mlout", [768, TC], BF16), "poolout": dint("x_poolout", [512, TC], BF16),
         "stblob": dint("x_stblob", [128, 772]), "hsel": dint("x_hsel", [128, 82]), "gS": dint("x_gS", [512, 772])}
    hbuf = [dint("x_hA", [D, TC]), dint("x_hB", [D, TC])]
    with contextlib.ExitStack() as es:
        S = Sched(nc, es)
        toks = []
        for l in range(NL):
            h_in = hT0 if l == 0 else hbuf[(l - 1) % 2]
            h_out = hN if l == NL - 1 else hbuf[l % 2]
            phase_p1(nc, S, h_in, w_in[l], g_pre[l], X)
            phase_gather1(nc, S, X)
            phase_attn(nc, S, X, Cst)
            phase_gather2(nc, S, X)
            phase_ml(nc, S, X, Cst, convw[l], gbias[l], pool_w[l], pscale[l])
            phase_unshuffle(nc, S, X, Cst)
            toks = phase_p3(nc, S, X, h_in, h_out, w_out[l], gains[l], w_gu[l], w_dn[l])
        S.barrier()
        S.emit()
    return nc

import ml_dtypes
_BF = ml_dtypes.bfloat16
_PROGS = {}


def _lay_g(g):
    return np.ascontiguousarray(np.asarray(g, np.float32).reshape(16, 128).T)


def _consts(q):
    j = np.arange(128)
    s = j[:, None]
    t = j[None, :]
    m4 = np.zeros((128, 4, 128), np.float32)
    for jj in range(4):
        if jj == q:
            m4[:, jj, :] = np.where(s < t, 0.0, -30000.0)
        elif jj > q:
            m4[:, jj, :] = -30000.0
    sm = np.arange(16)[:, None]
    mmeta = np.where((t >= 112) & (sm < t - 112), 0.0, -30000.0)
    ntri = np.where(s >= t, -1.0, 0.0)
    d = {"mask4": m4.astype(_BF), "maskm": mmeta.astype(_BF), "identb": np.eye(128).astype(_BF), "ntri": ntri.astype(_BF)}
    oh = np.zeros((128, 4), np.float32)
    oh[:, q] = 1.0
    d["selq"] = oh
    d["selq4"] = (oh * np.float32(QSCALE)).astype(np.float32)
    sp = np.zeros((128, 5), np.float32)
    if q == 0:
        sp[:, 4] = 1.0
    else:
        sp[:, q - 1] = 1.0
    d["selprev"] = sp
    d["triu"] = (s <= t).astype(np.float32)
    d["ones128"] = np.ones((128, 128), np.float32)
    d["i6"] = np.eye(6, dtype=np.float32)
    d["mask01"] = (s <= t).astype(np.float32)
    selh = np.zeros((6, 6, 128), np.float32)
    for hh in range(6):
        selh[hh, hh, :] = 1
    d["selh"] = selh
    ic = np.zeros((128, 4, 128), np.float32)
    for gg in range(4):
        w = 2 ** (gg + 1)
        ic[:, gg, :] = 1.0 / np.clip(j - 111, 1, w).astype(np.float32)
    d["invcnt"] = ic
    return d


def _shared_inputs(NL, w_in, ml_conv_w, ml_igate_b, ml_fgate_b, pool_w, pool_scale, w_out, g_mix_pre, g_mix_post, g_ffn_pre, g_ffn_post, w_gate_up, w_down):
    f = lambda a: np.ascontiguousarray(np.asarray(a, np.float32)[:NL])
    d = {"w_in": f(w_in), "w_out": f(w_out), "w_gu": f(w_gate_up), "w_dn": f(w_down), "pool_w": f(pool_w)}
    d["g_pre"] = np.stack([_lay_g(g_mix_pre[l]) for l in range(NL)])
    d["convw"] = np.stack([np.ascontiguousarray(np.asarray(ml_conv_w[l], np.float32).reshape(4, 6, 128).transpose(2, 1, 0)) for l in range(NL)])
    d["gbias"] = np.stack([np.stack([np.asarray(ml_igate_b[l], np.float32), np.asarray(ml_fgate_b[l], np.float32)], 1) for l in range(NL)])
    d["pscale"] = np.stack([np.ascontiguousarray(np.asarray(pool_scale[l], np.float32).T) for l in range(NL)])
    d["gains"] = np.stack([np.stack([_lay_g(g_mix_post[l]), _lay_g(g_ffn_pre[l]), _lay_g(g_ffn_post[l])], 1) for l in range(NL)])
    return {k: np.ascontiguousarray(v) for k, v in d.items()}


def run_fused(NL, x, meta_tokens, **params):
    x = np.asarray(x, np.float32)
    meta = np.asarray(meta_tokens, np.float32)
    if NL not in _PROGS:
        _PROGS[NL] = build_fused(NL)
    nc = _PROGS[NL]
    shared = _shared_inputs(NL, **params)
    ins = []
    for c in range(8):
        b, q = c // 4, c % 4
        h = np.zeros((TC, D), np.float32)
        h[112:128] = meta
        h[128:] = x[b, q * 2048:(q + 1) * 2048]
        d = dict(shared)
        d.update(_consts(q))
        d["hT0"] = np.ascontiguousarray(h.T)
        ins.append(d)
    res = run_bass_kernel_spmd(nc, ins, core_ids=list(range(8))).results
    out = np.zeros((2, 8192, D), np.float32)
    for c in range(8):
        b, q = c // 4, c % 4
        out[b, q * 2048:(q + 1) * 2048] = res[c]["hN"][:, 128:].T
    return out


def kernel(x, meta_tokens, w_in, ml_conv_w, ml_igate_b, ml_fgate_b, pool_w, pool_scale, w_out,
           g_mix_pre, g_mix_post, g_ffn_pre, g_ffn_post, w_gate_up, w_down):
    return run_fused(4, x, meta_tokens, w_in=w_in, ml_conv_w=ml_conv_w, ml_igate_b=ml_igate_b, ml_fgate_b=ml_fgate_b, pool_w=pool_w,
                     pool_scale=pool_scale, w_out=w_out, g_mix_pre=g_mix_pre, g_mix_post=g_mix_post, g_ffn_pre=g_ffn_pre,
                     g_ffn_post=g_ffn_post, w_gate_up=w_gate_up, w_down=w_down)
```

```python
import contextlib
import numpy as np
import concourse.bass as bass
import concourse.mybir as mybir
from concourse.bass_utils import run_bass_kernel_spmd

F32 = mybir.dt.float32
BF16 = mybir.dt.bfloat16
AF = mybir.ActivationFunctionType
ALU = mybir.AluOpType

ENGS = ("pe", "act", "dve", "pool", "sp")
NDMA = 6


class Sched:
    def __init__(self, nc, es):
        self.nc = nc
        self.es = es
        self.q = {e: [] for e in ENGS}
        self.sem = {}
        self.cnt = {}
        for e in ("pe", "act", "dve", "pool"):
            self.sem[e] = es.enter_context(nc.semaphore("s_" + e))
            self.cnt[e] = 0
        self.dring = {}
        for e in ("sp", "pool", "act"):
            ring = []
            for i in range(NDMA):
                nm = "d_%s%d" % (e, i)
                self.sem[nm] = es.enter_context(nc.semaphore(nm))
                self.cnt[nm] = 0
                ring.append(nm)
            self.dring[e] = [ring, 0]
        self.sem["cc"] = es.enter_context(nc.semaphore("s_cc"))
        self.cnt["cc"] = 0
        self.waited = {e: {} for e in ENGS}
        self.last_w = {}
        self.readers = {}
        self.nops = 0

    def _deps(self, reads, writes):
        deps = []
        for k in reads:
            t = self.last_w.get(k)
            if t is not None:
                deps.append(t)
        for k in writes:
            t = self.last_w.get(k)
            if t is not None:
                deps.append(t)
            deps.extend(self.readers.get(k, ()))
        return deps

    def _commit(self, tok, reads, writes):
        for k in reads:
            self.readers.setdefault(k, []).append(tok)
        for k in writes:
            self.last_w[k] = tok
            self.readers[k] = []

    def _waits(self, eng, deps, skip_self=False):
        best = {}
        for (s, v) in deps:
            if skip_self and s == eng:
                continue
            if best.get(s, 0) < v:
                best[s] = v
        out = []
        w = self.waited[eng]
        for s, v in best.items():
            if w.get(s, 0) < v:
                w[s] = v
                out.append((s, v))
        return out

    def op(self, eng, fn, reads=(), writes=(), deps=()):
        d = self._deps(reads, writes) + list(deps)
        waits = self._waits(eng, d, skip_self=(eng == "pe"))
        self.cnt[eng] += 1
        tok = (eng, self.cnt[eng])
        self.q[eng].append((waits, fn, (eng, 1)))
        self._commit(tok, reads, writes)
        self.nops += 1
        return tok

    def dma(self, fn, reads=(), writes=(), queue="sp", deps=()):
        ring, idx = self.dring[queue]
        nm = ring[idx % NDMA]
        self.dring[queue][1] = idx + 1
        d = self._deps(reads, writes) + list(deps)
        if self.cnt[nm] > 0:
            d.append((nm, self.cnt[nm]))
        waits = self._waits(queue, d)
        self.cnt[nm] += 16
        tok = (nm, self.cnt[nm])
        self.q[queue].append((waits, fn, (nm, 16)))
        self._commit(tok, reads, writes)
        self.nops += 1
        return tok

    def cc(self, fn, reads=(), writes=(), inc=1):
        d = self._deps(reads, writes)
        waits = self._waits("pool", d)
        self.cnt["cc"] += inc
        tok = ("cc", self.cnt["cc"])
        self.q["pool"].append((waits, fn, ("cc", inc)))
        self._commit(tok, reads, writes)
        return tok

    def barrier(self):
        allt = [(s, v) for s, v in self.cnt.items() if v > 0]
        for e in ENGS:
            waits = self._waits(e, allt)
            if waits:
                self.q[e].append((waits, None, None))
        self.last_w = {}
        self.readers = {}

    def emit(self, final_tokens=()):
        nc = self.nc
        waits = self._waits("sp", list(final_tokens))
        if waits:
            self.q["sp"].append((waits, None, None))
        sem = self.sem
        with nc.Block() as block:
            def runner(name):
                def run(eng):
                    for (waits, fn, inc) in self.q[name]:
                        for (s, v) in waits:
                            eng.wait_ge(sem[s], v)
                        if fn is not None:
                            fn(eng).then_inc(sem[inc[0]], inc[1])
                return run
            block.tensor(runner("pe"))
            block.scalar(runner("act"))
            block.vector(runner("dve"))
            block.gpsimd(runner("pool"))
            block.sync(runner("sp"))
        self.q = {e: [] for e in ENGS}


D = 2048
TC = 17 * 128
NK = 16
EPS = 1e-6
TT = [(0, 512), (512, 512), (1024, 512), (1536, 512), (2048, 128)]
IN_W = 5132
FF = 5632
NKEY = 16 + 8192
QSCALE = float(1.0 / np.sqrt(128.0))
LN8 = float(np.log(8.0))
GROUPS = [[0, 1, 2, 3], [4, 5, 6, 7]]
FM_GROUPS = [
    ("qkpre", 0, 384, 0), ("qkpre", 384, 384, 384),
    ("mlo", 1536, 384, 0), ("mlo", 1920, 384, 384),
    ("gates", 2304, 12, 0),
    ("sbq", 2316, 384, 0), ("sbq", 2700, 384, 384),
    ("sbk", 3084, 384, 0), ("sbk", 3468, 384, 384),
    ("poolu", 4620, 512, 0),
]
TM_GROUPS = [("mlv", 768, 384, 0), ("mlv", 1152, 384, 3), ("sbv", 3852, 384, 0), ("sbv", 4236, 384, 3)]


class Ctx:
    uid = [0]

    def __init__(self, nc, S):
        self.nc = nc
        self.S = S
        self.es = contextlib.ExitStack()
        Ctx.uid[0] += 1
        self.pfx = "p%d_" % Ctx.uid[0]

    def sb(self, name, shape, dt):
        return self.es.enter_context(self.nc.sbuf_tensor(self.pfx + name, list(shape), dt))

    def ps(self, name, shape=(128, 512), dt=F32):
        return self.es.enter_context(self.nc.psum_tensor(self.pfx + name, list(shape), dt))

    def close(self):
        self.S.barrier()
        self.S.emit()
        self.es.close()


def emit_rstd(C, src, srck, sq, sqk, rr, ones_bf, bank, bankk, tn):
    S = C.S
    S.op("act", lambda e: e.activation(sq[:, :, 0:tn], src[:, :, 0:tn], AF.Square), reads=[srck], writes=[sqk])
    for k in range(NK):
        S.op("pe", lambda e, k=k: e.matmul(bank[:, 0:tn], ones_bf[:], sq[:, k, 0:tn], start=(k == 0), stop=(k == NK - 1)), reads=[sqk, "ones"], writes=[bankk])
    S.op("act", lambda e: e.activation(rr[:, 0:tn], bank[:, 0:tn], AF.Sqrt, bias=EPS, scale=1.0 / D), reads=[bankk], writes=["rr"])
    S.op("dve", lambda e: e.reciprocal(rr[:, 0:tn], rr[:, 0:tn]), reads=["rr"], writes=["rr"])


def phase_p1(nc, S, hT, w_in_l, g_pre_l, X):
    C = Ctx(nc, S)
    banks = [C.ps("bank%d" % i) for i in range(8)]
    ones_bf = C.sb("ones", [128, 128], BF16)
    gs = C.sb("gs", [128, NK], F32)
    xn = C.sb("xn", [128, NK, TC], BF16)
    ht = C.sb("rn_h", [128, NK, 512], F32)
    sq = C.sb("rn_sq", [128, NK, 512], BF16)
    rr = C.sb("rn_r", [128, 512], F32)
    S.op("pool", lambda e: e.memset(ones_bf[:], 1.0), writes=["ones"])
    S.dma(lambda e: e.dma_start(out=gs[:], in_=g_pre_l), writes=["gs"])
    hv = hT.rearrange("(k p) t -> p k t", p=128)

    def norm_tile(t0, tn):
        S.dma(lambda e: e.dma_start(out=ht[:, :, 0:tn], in_=hv[:, :, t0:t0 + tn]), writes=["rn_h"])
        emit_rstd(C, ht, "rn_h", sq, "rn_sq", rr, ones_bf, banks[7], "bank7", tn)
        for k in range(NK):
            S.op("dve", lambda e, k=k: e.scalar_tensor_tensor(xn[:, k, t0:t0 + tn], ht[:, k, 0:tn], gs[:, k:k + 1], rr[:, 0:tn], ALU.mult, ALU.mult),
                 reads=["rn_h", "rr", "gs"], writes=["xn%d" % (t0 // 512)])
        if t0 == 0:
            S.op("pool", lambda e: e.memset(xn[:, :, 0:112], 0.0), writes=["xn0"])
    for (t0, tn) in TT:
        norm_tile(t0, tn)
    xkeys = ["xn%d" % i for i in range(5)]
    wv = w_in_l.rearrange("(k p) e -> p k e", p=128)
    wb = [C.sb("wb%d" % i, [128, NK, 512], BF16) for i in range(2)]
    odt = {"qkpre": F32, "mlo": BF16, "gates": F32, "sbq": BF16, "sbk": BF16, "poolu": F32}
    stg = {F32: [C.sb("stf%d" % i, [128, 512], F32) for i in range(3)], BF16: [C.sb("stb%d" % i, [128, 512], BF16) for i in range(3)]}
    sti = {F32: 0, BF16: 0}
    gi = 0
    bi = 0
    for (name, c0, ncol, r0) in FM_GROUPS:
        w = wb[gi % 2]
        wk = "wb%d" % (gi % 2)
        gi += 1
        S.dma(lambda e, w=w, c0=c0, ncol=ncol: e.dma_start(out=w[:, :, 0:ncol], in_=wv[:, :, c0:c0 + ncol]), writes=[wk], queue="pool")
        dt = odt[name]
        for ti, (t0, tn) in enumerate(TT):
            for j0 in range(0, ncol, 128):
                jn = min(128, ncol - j0)
                bk = bi % 6
                bi += 1
                pb = banks[bk]
                for k in range(NK):
                    S.op("pe", lambda e, pb=pb, w=w, k=k, j0=j0, jn=jn, t0=t0, tn=tn: e.matmul(
                        pb[0:jn, 0:tn], w[:, k, j0:j0 + jn], xn[:, k, t0:t0 + tn], start=(k == 0), stop=(k == NK - 1)),
                        reads=[wk, xkeys[ti]], writes=["bank%d" % bk])
                si = sti[dt] % 3
                sti[dt] += 1
                st = stg[dt][si]
                sk = "st%s%d" % ("f" if dt == F32 else "b", si)
                if name == "mlo":
                    S.op("act", lambda e, st=st, pb=pb, jn=jn, tn=tn: e.activation(st[0:jn, 0:tn], pb[0:jn, 0:tn], AF.Sigmoid), reads=["bank%d" % bk], writes=[sk])
                else:
                    S.op("dve", lambda e, st=st, pb=pb, jn=jn, tn=tn: e.tensor_copy(st[0:jn, 0:tn], pb[0:jn, 0:tn]), reads=["bank%d" % bk], writes=[sk])
                o = X[name]
                S.dma(lambda e, o=o, st=st, r=r0 + j0, jn=jn, t0=t0, tn=tn: e.dma_start(out=o[r:r + jn, t0:t0 + tn], in_=st[0:jn, 0:tn]), reads=[sk], writes=["o_" + name])
    for (name, c0, ncol, h0) in TM_GROUPS:
        w = wb[gi % 2]
        wk = "wb%d" % (gi % 2)
        gi += 1
        S.dma(lambda e, w=w, c0=c0, ncol=ncol: e.dma_start(out=w[:, :, 0:ncol], in_=wv[:, :, c0:c0 + ncol]), writes=[wk], queue="pool")
        for b in range(17):
            bk = bi % 6
            bi += 1
            pb = banks[bk]
            for k in range(NK):
                S.op("pe", lambda e, pb=pb, w=w, k=k, b=b, ncol=ncol: e.matmul(
                    pb[:, 0:ncol], xn[:, k, b * 128:(b + 1) * 128], w[:, k, 0:ncol], start=(k == 0), stop=(k == NK - 1)),
                    reads=[wk, xkeys[min(b // 4, 4)]], writes=["bank%d" % bk])
            si = sti[BF16] % 3
            sti[BF16] += 1
            st = stg[BF16][si]
            sk = "stb%d" % si
            S.op("dve", lambda e, st=st, pb=pb, ncol=ncol: e.tensor_copy(st[:, 0:ncol], pb[:, 0:ncol]), reads=["bank%d" % bk], writes=[sk])
            o = X[name]
            for hh in range(3):
                S.dma(lambda e, o=o, st=st, b=b, hh=hh, h0=h0: e.dma_start(out=o[h0 + hh, b * 128:(b + 1) * 128, :], in_=st[:, hh * 128:(hh + 1) * 128]),
                      reads=[sk], writes=["o_" + name])
    C.close()
    C = Ctx(nc, S)
    tl = C.sb("tl", [128, 82], F32)
    S.dma(lambda e: e.dma_start(out=tl[:, 0:18].rearrange("p (c j) -> p c j", j=3), in_=X["qkpre"].rearrange("(c p) t -> p c t", p=128)[:, :, TC - 3:TC]), writes=["tl"])
    S.dma(lambda e: e.dma_start(out=tl[:, 18:82].rearrange("p (g j) -> p g j", j=16), in_=X["poolu"].rearrange("(g p) t -> p g t", p=128)[:, :, TC - 16:TC]), writes=["tl"])
    S.dma(lambda e: e.dma_start(out=X["tail"][:, :], in_=tl[:]), reads=["tl"], writes=["tail"])
    C.close()


def phase_gather1(nc, S, X):
    S.cc(lambda e: e.collective_compute("AllGather", ALU.bypass, replica_groups=GROUPS, ins=[X["tail"][:, :].opt()], outs=[X["gT"][:, :].opt()]), writes=["gT"])
    for h in range(6):
        S.cc(lambda e, h=h: e.collective_compute("AllGather", ALU.bypass, replica_groups=GROUPS, ins=[X["sbk"][h * 128:(h + 1) * 128, :].opt()], outs=[X["gK"][h].opt()]), writes=["gK%d" % h])
        S.cc(lambda e, h=h: e.collective_compute("AllGather", ALU.bypass, replica_groups=GROUPS, ins=[X["sbq"][h * 128:(h + 1) * 128, :].opt()], outs=[X["gQ"][h].opt()]), writes=["gQ%d" % h])
        S.cc(lambda e, h=h: e.collective_compute("AllGather", ALU.bypass, replica_groups=GROUPS, ins=[X["sbv"][h].opt()], outs=[X["gV"][h].opt()]), writes=["gV%d" % h])


def phase_attn(nc, S, X, Cst):
    C = Ctx(nc, S)
    sbo = X["sbo_c"]
    zb = [C.ps("zb%d" % i) for i in range(2)]
    ab = [C.ps("ab%d" % i) for i in range(2)]
    cb = [C.ps("cb%d" % i) for i in range(2)]
    ob = [C.ps("ob%d" % i) for i in range(2)]
    m4 = C.sb("m4", [128, 4, 128], BF16)
    mm = C.sb("mm", [16, 128], BF16)
    idt = C.sb("idt", [128, 128], BF16)
    ntr = C.sb("ntr", [128, 128], BF16)
    onec = C.sb("onec", [128, 1], BF16)
    negr = C.sb("negr", [1, 128], BF16)
    sq4 = C.sb("sq4", [128, 4], F32)
    S.dma(lambda e: e.dma_start(out=m4[:], in_=Cst["mask4"][:, :, :]), writes=["consts"])
    S.dma(lambda e: e.dma_start(out=mm[:], in_=Cst["maskm"][:, :]), writes=["consts"])
    S.dma(lambda e: e.dma_start(out=idt[:], in_=Cst["identb"][:, :]), writes=["consts"])
    S.dma(lambda e: e.dma_start(out=ntr[:], in_=Cst["ntri"][:, :]), writes=["consts"])
    S.dma(lambda e: e.dma_start(out=sq4[:], in_=Cst["selq4"][:, :]), writes=["sq4"])
    S.op("dve", lambda e: e.memset(onec[:], 1.0), writes=["consts2"])
    S.op("dve", lambda e: e.memset(negr[:], -1.0), writes=["consts2"])
    Kh = [C.sb("Kh%d" % i, [128, NKEY], BF16) for i in range(2)]
    Vh = [C.sb("Vh%d" % i, [128, 64, 128], BF16) for i in range(2)]
    Vm = [C.sb("Vm%d" % i, [16, 128], BF16) for i in range(2)]
    Qh = [C.sb("Qh%d" % i, [128, TC], BF16) for i in range(2)]
    Qa = C.sb("Qa", [128, 8192], BF16)
    ee = [C.sb("ee%d" % i, [128, 512], F32) for i in range(2)]
    sp = [C.sb("sp%d" % i, [128, 512], BF16) for i in range(2)]
    AA = [C.sb("AA%d" % i, [128, 512], BF16) for i in range(2)]
    cf = [[C.sb("cf%d_%d" % (i, j), [1, 5, 128], F32) for j in range(2)] for i in range(2)]
    crow = [C.sb("crow%d" % i, [1, 4, 128], BF16) for i in range(2)]
    osb = [C.sb("osb%d" % i, [128, 128], BF16) for i in range(2)]
    cfp = [C.sb("cfp%d" % i, [1, 128], F32) for i in range(2)]
    cpb = [C.sb("cpb%d" % i, [1, 128], BF16) for i in range(2)]
    nones = C.sb("nones", [128, 128], BF16)
    S.op("dve", lambda e: e.memset(nones[:], -1.0), writes=["consts2"])

    class Stream:
        pass

    def run_slot_pair(hb, h, slots):
        K, V, VM, Q = Kh[hb], Vh[hb], Vm[hb], Qh[hb]
        kk = ["Kh%d" % hb, "Vh%d" % hb, "Qh%d" % hb]
        sts = []
        for (si, a) in slots:
            st = Stream()
            st.si = si
            st.a = a
            st.qc = 0 if a < 0 else (1 + a) * 128
            if a < 0:
                st.groups = [("metaq", None)]
            else:
                st.groups = [("mask", [4 * a + 3, 4 * a + 2, 4 * a + 1, 4 * a])]
                for g in range(a - 1, -1, -1):
                    st.groups.append(("live", [4 * g + 3, 4 * g + 2, 4 * g + 1, 4 * g]))
                st.groups.append(("meta", None))
            st.cfi = 0
            st.first_av = True
            sts.append(st)
        ng = max(len(st.groups) for st in sts)

        def qk(st, dst, dkey, kind, blocks, start_first):
            q = Q[:, st.qc:st.qc + 128]
            if kind in ("meta", "metaq"):
                S.op("pe", lambda e: e.matmul(dst[0:16, 0:128], K[:, 0:16], q, start=True, stop=(kind == "meta")), reads=kk, writes=[dkey])
                if kind == "metaq":
                    S.op("pe", lambda e: e.matmul(dst[0:16, 0:128], idt[0:16, 0:16], mm[:, :], start=False, stop=True), reads=["consts"], writes=[dkey])
                return
            for n, blk in enumerate(blocks):
                kc = 16 + blk * 128
                S.op("pe", lambda e, n=n, kc=kc: e.matmul(dst[:, n * 128:(n + 1) * 128], K[:, kc:kc + 128], q, start=True, stop=(kind != "mask")),
                     reads=kk, writes=[dkey])
                if kind == "mask":
                    jj = 3 - n
                    S.op("pe", lambda e, n=n, jj=jj: e.matmul(dst[:, n * 128:(n + 1) * 128], idt[:], m4[:, jj, :], start=False, stop=True),
                         reads=["consts"], writes=[dkey])

        def stage1(st, gi):
            kind, blocks = st.groups[gi]
            qk(st, zb[st.si], "zb%d" % st.si, kind, blocks, True)

        def stage2(st, gi):
            kind, blocks = st.groups[gi]
            si = st.si
            z = zb[si]
            np_, nf = (16, 128) if kind in ("meta", "metaq") else (128, 512)
            S.op("act", lambda e: e.activation(ee[si][0:np_, 0:nf], z[0:np_, 0:nf], AF.Exp), reads=["zb%d" % si], writes=["ee%d" % si])
            S.op("act", lambda e: e.activation(sp[si][0:np_, 0:nf], ee[si][0:np_, 0:nf], AF.Ln, bias=1.0, scale=1.0), reads=["ee%d" % si], writes=["sp%d" % si])

        def stage3(st, gi):
            kind, blocks = st.groups[gi]
            si = st.si
            a_ = ab[si]
            ak = "ab%d" % si
            q = Q[:, st.qc:st.qc + 128]
            ck = "cpb%d" % si
            if kind in ("meta", "metaq"):
                S.op("pe", lambda e: e.matmul(a_[0:16, 0:128], ntr[0:16, 0:16], sp[si][0:16, 0:128], start=True, stop=False), reads=["sp%d" % si, "consts"], writes=[ak])
                S.op("pe", lambda e: e.matmul(a_[0:16, 0:128], K[:, 0:16], q, start=False, stop=False), reads=kk, writes=[ak])
                if kind == "metaq":
                    S.op("pe", lambda e: e.matmul(a_[0:16, 0:128], idt[0:16, 0:16], mm[:, :], start=False, stop=True), reads=["consts"], writes=[ak])
                else:
                    S.op("pe", lambda e: e.matmul(a_[0:16, 0:128], negr[0:1, 0:16], cpb[si][0:1, :], start=False, stop=True), reads=[ck, "consts2"], writes=[ak])
                return
            S.op("pe", lambda e: e.matmul(a_[:, 0:512], ntr[:], sp[si][:, 0:512], start=True, stop=False), reads=["sp%d" % si, "consts"], writes=[ak])
            for n, blk in enumerate(blocks):
                kc = 16 + blk * 128
                dst = a_[:, n * 128:(n + 1) * 128]
                S.op("pe", lambda e, kc=kc, dst=dst: e.matmul(dst, K[:, kc:kc + 128], q, start=False, stop=False), reads=kk, writes=[ak])
                if kind == "mask":
                    S.op("pe", lambda e, dst=dst, jj=3 - n: e.matmul(dst, idt[:], m4[:, jj, :], start=False, stop=False), reads=["consts"], writes=[ak])
            for m in range(3):
                src = sp[si][:, m * 128:(m + 1) * 128]
                rep = bass.AP(src.tensor, src.offset, [list(src.ap[0]), [0, 3 - m], list(src.ap[1])])
                dstv = a_[:, (m + 1) * 128:512].rearrange("p (r t) -> p r t", t=128)
                S.op("pe", lambda e, rep=rep, dstv=dstv: e.matmul(dstv, nones[:], rep, start=False, stop=False), reads=["sp%d" % si, "consts2"], writes=[ak])
            crep = bass.AP(cpb[si][0:1, :].tensor, cpb[si][0:1, :].offset, [list(cpb[si][0:1, :].ap[0]), [0, 4], list(cpb[si][0:1, :].ap[1])])
            S.op("pe", lambda e, crep=crep: e.matmul(a_[:, 0:512].rearrange("p (r t) -> p r t", t=128), negr[0:1, 0:128], crep, start=False, stop=True),
                 reads=[ck, "consts2"], writes=[ak])
            for n in range(4):
                S.op("pe", lambda e, n=n: e.matmul(cb[si][0:1, 0:128], onec[:, 0:1], sp[si][:, n * 128:(n + 1) * 128], start=(n == 0), stop=(n == 3)),
                     reads=["sp%d" % si, "consts2"], writes=["cb%d" % si])
            S.op("dve", lambda e: e.tensor_tensor(cfp[si][0:1, :], cfp[si][0:1, :], cb[si][0:1, 0:128], ALU.add), reads=["cfp%d" % si, "cb%d" % si], writes=["cfp%d" % si])
            S.op("dve", lambda e: e.tensor_copy(cpb[si][0:1, :], cfp[si][0:1, :]), reads=["cfp%d" % si], writes=[ck])

        def stage4(st, gi):
            kind, blocks = st.groups[gi]
            si = st.si
            np_, nf = (16, 128) if kind in ("meta", "metaq") else (128, 512)
            S.op("act", lambda e: e.activation(AA[si][0:np_, 0:nf], ab[si][0:np_, 0:nf], AF.Exp), reads=["ab%d" % si], writes=["AA%d" % si])

        def stage5(st, gi):
            kind, blocks = st.groups[gi]
            si = st.si
            o = ob[si][:, 0:128]
            last = (gi == len(st.groups) - 1)
            if kind in ("meta", "metaq"):
                S.op("pe", lambda e, fa=st.first_av: e.matmul(o, VM[0:16, :], AA[si][0:16, 0:128], start=fa, stop=True), reads=["AA%d" % si] + kk, writes=["ob%d" % si])
                st.first_av = False
            else:
                for n, blk in enumerate(blocks):
                    S.op("pe", lambda e, n=n, blk=blk, fa=st.first_av: e.matmul(o, V[:, blk, :], AA[si][:, n * 128:(n + 1) * 128], start=fa, stop=False),
                         reads=["AA%d" % si] + kk, writes=["ob%d" % si])
                    st.first_av = False
            if last:
                S.op("dve", lambda e: e.tensor_copy(osb[si][:], o), reads=["ob%d" % si], writes=["osb%d" % si])
                S.dma(lambda e, qc=st.qc: e.dma_start(out=sbo[h * 128:(h + 1) * 128, qc:qc + 128], in_=osb[si][:]), reads=["osb%d" % si], writes=["sbo"])

        for st in sts:
            si = st.si
            S.op("dve", lambda e, si=si: e.memset(cfp[si][0:1, :], 0.0), writes=["cfp%d" % si])
            S.op("dve", lambda e, si=si: e.memset(cpb[si][0:1, :], 0.0), writes=["cpb%d" % si])
            stage1(st, 0)
        for gi in range(ng):
            act = [st for st in sts if gi < len(st.groups)]
            for st in act:
                stage2(st, gi)
            for st in sts:
                if gi + 1 < len(st.groups):
                    stage1(st, gi + 1)
            for st in act:
                stage3(st, gi)
            for st in act:
                stage4(st, gi)
            for st in act:
                stage5(st, gi)

    for h in range(6):
        hb = h % 2
        K_, V_, Q_ = Kh[hb], Vh[hb], Qh[hb]
        S.dma(lambda e, K_=K_, h=h: e.dma_start(out=K_[:, 0:16], in_=X["sbk"][h * 128:(h + 1) * 128, 112:128]), writes=["Kh%d" % hb])
        S.dma(lambda e, hb=hb, h=h: e.dma_start(out=Vm[hb][:], in_=X["sbv"][h, 112:128, :]), writes=["Vh%d" % hb])
        for r in range(4):
            S.dma(lambda e, K_=K_, h=h, r=r: e.dma_start(out=K_[:, 16 + r * 2048:16 + (r + 1) * 2048], in_=X["gK"][h, r * 128:(r + 1) * 128, 128:TC]), reads=["gK%d" % h], writes=["Kh%d" % hb])
            S.dma(lambda e, V_=V_, h=h, r=r: e.dma_start(out=V_[:, r * 16:(r + 1) * 16, :], in_=X["gV"][h, r * TC + 128:(r + 1) * TC, :].rearrange("(n p) e -> p n e", p=128)),
                  reads=["gV%d" % h], writes=["Vh%d" % hb])
            S.dma(lambda e, h=h, r=r: e.dma_start(out=Qa[:, r * 2048:(r + 1) * 2048], in_=X["gQ"][h, r * 128:(r + 1) * 128, 128:TC]), reads=["gQ%d" % h], writes=["Qa"])
        S.dma(lambda e, Q_=Q_, h=h: e.dma_start(out=Q_[:, 0:128], in_=X["sbq"][h * 128:(h + 1) * 128, 0:128]), writes=["Qh%d" % hb])
        S.op("dve", lambda e, Q_=Q_: e.tensor_scalar(Q_[:, 0:128], Q_[:, 0:128], QSCALE, None, ALU.mult), reads=["Qh%d" % hb], writes=["Qh%d" % hb])
        qsel = Q_[:, 128:TC].rearrange("p (a t) -> p a t", t=128)
        qall = Qa[:].rearrange("p (a j t) -> p a j t", j=4, t=128)
        S.op("dve", lambda e, qsel=qsel, qall=qall: e.tensor_scalar(qsel, qall[:, :, 0, :], sq4[:, 0:1], None, ALU.mult), reads=["Qa", "sq4"], writes=["Qh%d" % hb])
        for jj in range(1, 4):
            S.op("dve", lambda e, qsel=qsel, qall=qall, jj=jj: e.scalar_tensor_tensor(qsel, qall[:, :, jj, :], sq4[:, jj:jj + 1], qsel, ALU.mult, ALU.add),
                 reads=["Qa", "sq4", "Qh%d" % hb], writes=["Qh%d" % hb])
        run_slot_pair(hb, h, [(0, -1)])
        for m in range(8):
            run_slot_pair(hb, h, [(0, 15 - m), (1, m)])
    C.close()


def phase_ml(nc, S, X, Cst, convw_l, gbias_l, pool_w_l, pscale_l):
    C = Ctx(nc, S)
    pg = C.ps("pg"); pbrow = C.ps("pbrow"); pT = C.ps("pT", (128, 512), BF16); pS = C.ps("pS"); pN = C.ps("pN"); pD = C.ps("pD")
    pE = C.ps("pE"); pC = C.ps("pC")
    triu = C.sb("triu", [128, 128], F32); ones = C.sb("ones", [128, 128], F32); i6 = C.sb("i6", [6, 6], F32); selh = C.sb("selh", [6, 6, 128], F32)
    mask01 = C.sb("mask01", [128, 128], F32); identb = C.sb("identb", [128, 128], BF16)
    cw = C.sb("cw", [128, 6, 4], F32); gb = C.sb("gb", [6, 2], F32); ngb = C.sb("ngb", [6, 1], F32)
    selp = C.sb("selp", [128, 5], F32); selq = C.sb("selq", [128, 4], F32)
    for (t, d, k) in [(triu, Cst["triu"], "triu"), (ones, Cst["ones128"], "ones"), (i6, Cst["i6"], "i6"), (mask01, Cst["mask01"], "mask01"),
                      (identb, Cst["identb"], "identb"), (selp, Cst["selprev"], "selp"), (selq, Cst["selq"], "selq")]:
        S.dma(lambda e, t=t, d=d: e.dma_start(out=t[:], in_=d[:, :]), writes=[k])
    S.dma(lambda e: e.dma_start(out=gb[:], in_=gbias_l), writes=["gb"])
    S.dma(lambda e: e.dma_start(out=selh[:], in_=Cst["selh"][:, :, :]), writes=["selh"])
    S.dma(lambda e: e.dma_start(out=cw[:], in_=convw_l), writes=["cw"])
    S.op("dve", lambda e: e.tensor_scalar(ngb[:], gb[:, 1:2], -1.0, None, ALU.mult), reads=["gb"], writes=["ngb"])

    gT = C.sb("gT", [128, 4, 82], F32); mt = C.sb("mt", [128, 82], F32); hb_ = C.sb("hb", [128, 82], F32)
    S.dma(lambda e: e.dma_start(out=gT[:], in_=X["gT"].rearrange("(r p) x -> p r x", p=128)), reads=["gT"], writes=["sgT"])
    S.dma(lambda e: e.dma_start(out=mt[:, 0:18].rearrange("p (c j) -> p c j", j=3), in_=X["qkpre"].rearrange("(c p) t -> p c t", p=128)[:, :, 125:128]), writes=["mt"])
    S.dma(lambda e: e.dma_start(out=mt[:, 18:82].rearrange("p (g j) -> p g j", j=16), in_=X["poolu"].rearrange("(g p) t -> p g t", p=128)[:, :, 112:128]), writes=["mt"])
    S.op("dve", lambda e: e.tensor_scalar(hb_[:], mt[:], selp[:, 4:5], None, ALU.mult), reads=["mt", "selp"], writes=["hb"])
    for r in range(4):
        S.op("dve", lambda e, r=r: e.scalar_tensor_tensor(hb_[:], gT[:, r, :], selp[:, r:r + 1], hb_[:], ALU.mult, ALU.add), reads=["sgT", "selp", "hb"], writes=["hb"])

    S.dma(lambda e: e.dma_start(out=X["hsel"][:, :], in_=hb_[:]), reads=["hb"], writes=["hsel"])

    qk = C.sb("qk", [128, 6, TC], BF16)
    XW = 3 + 128 + 3 + 2048
    xb = [C.sb("xb0", [128, XW], F32)] * 2
    yb = [C.sb("yb0", [128, TC], F32)] * 2
    for c in range(6):
        x = xb[0]; y = yb[0]; xk = "xb0"; yk = "yb0"
        S.op("pool", lambda e, x=x: e.memset(x[:, 0:3], 0.0), writes=[xk])
        S.dma(lambda e, x=x, c=c: e.dma_start(out=x[:, 3:131], in_=X["qkpre"][c * 128:(c + 1) * 128, 0:128]), writes=[xk])
        S.op("pool", lambda e, x=x, c=c: e.tensor_copy(x[:, 131:134], hb_[:, c * 3:(c + 1) * 3]), reads=["hb"], writes=[xk])
        S.dma(lambda e, x=x, c=c: e.dma_start(out=x[:, 134:XW], in_=X["qkpre"][c * 128:(c + 1) * 128, 128:TC]), writes=[xk])
        for (x0, y0, n) in [(0, 0, 128), (131, 128, 2048)]:
            S.op("dve", lambda e, x=x, y=y, c=c, x0=x0, y0=y0, n=n: e.tensor_scalar(y[:, y0:y0 + n], x[:, x0 + 3:x0 + 3 + n], cw[:, c, 3:4], None, ALU.mult),
                 reads=[xk, "cw"], writes=[yk])
            for j in range(3):
                S.op("dve", lambda e, x=x, y=y, c=c, x0=x0, y0=y0, n=n, j=j: e.scalar_tensor_tensor(
                    y[:, y0:y0 + n], x[:, x0 + j:x0 + j + n], cw[:, c, j:j + 1], y[:, y0:y0 + n], ALU.mult, ALU.add), reads=[xk, "cw", yk], writes=[yk])
        S.op("act", lambda e, y=y, c=c: e.activation(qk[:, c, :], y[:], AF.Silu), reads=[yk], writes=["qk"])

    gi_ = C.sb("gi", [6, TC], F32); gf_ = C.sb("gf", [6, TC], F32)
    S.dma(lambda e: e.dma_start(out=gi_[:], in_=X["gates"][0:6, :]), writes=["gi"])
    S.dma(lambda e: e.dma_start(out=gf_[:], in_=X["gates"][6:12, :]), writes=["gf"])
    S.op("act", lambda e: e.activation(gi_[:], gi_[:], AF.Identity, bias=gb[:, 0:1], scale=1.0), reads=["gi", "gb"], writes=["gi"])
    S.op("pool", lambda e: e.memset(gi_[:, 0:112], -30000.0), reads=["gi"], writes=["gi"])
    S.op("act", lambda e: e.activation(gf_[:], gf_[:], AF.Exp, bias=ngb[:, 0:1], scale=-1.0), reads=["gf", "ngb"], writes=["gf"])
    S.op("act", lambda e: e.activation(gf_[:], gf_[:], AF.Ln, bias=1.0, scale=1.0), reads=["gf"], writes=["gf"])
    S.op("pool", lambda e: e.memset(gf_[:, 0:112], 0.0), reads=["gf"], writes=["gf"])

    Va = C.sb("Va", [128, 17, 6, 256], BF16)
    S.op("pool", lambda e: e.memset(Va[:, :, :, 128:256], 1.0), writes=["Va1"])
    for h in range(6):
        S.dma(lambda e, h=h: e.dma_start(out=Va[:, :, h, 0:128], in_=X["mlv"][h].rearrange("(n p) v -> p n v", p=128)), writes=["Va0"])
    so = C.sb("so", [128, 6, TC], BF16)
    S.dma(lambda e: e.dma_start(out=so[:], in_=X["mlo"].rearrange("(h p) t -> p h t", p=128)), writes=["so"])

    Sst = C.sb("Sst", [128, 3, 256], F32); Sbf = C.sb("Sbf", [128, 3, 256], BF16); Sin = C.sb("Sin", [128, 3, 256], F32)
    Mst = C.sb("Mst", [128, 3, 256], F32); blob = C.sb("blob", [128, 772], F32); G = C.sb("G", [128, 4, 772], F32)
    lfT = C.sb("lfT", [128, 6], F32); iT = C.sb("iT", [128, 6], F32); tmp6 = C.sb("tmp6", [128, 6], F32); wcol = C.sb("wcol", [128, 6], F32)
    EcolA = C.sb("EcolA", [128, 17, 6], F32); eblA = C.sb("eblA", [128, 17, 6], F32); ebrA = C.sb("ebrA", [6, 17, 128], F32)
    wkA = C.sb("wkA", [128, 17, 3, 128], BF16)
    Sw = C.sb("Sw", [128, 128], BF16)
    aden = C.sb("aden", [128, 128], F32); hm = C.sb("hm", [128, 128], F32)
    mo = [C.sb("mo%d" % i, [128, 128], BF16) for i in range(2)]
    moi = [0]

    def chunk_gates(c):
        tsl = slice(c * 128, (c + 1) * 128)
        Ecol = EcolA[:, c, :]; ebl = eblA[:, c, :]
        S.op("pe", lambda e: e.matmul(pg[:, 0:6], gf_[:, tsl], i6[:], start=True, stop=True), reads=["gf", "i6"], writes=["pg"])
        S.op("pe", lambda e: e.matmul(pg[:, 6:12], gi_[:, tsl], i6[:], start=True, stop=True), reads=["gi", "i6"], writes=["pg"])
        S.op("dve", lambda e: e.tensor_copy(lfT[:], pg[:, 0:6]), reads=["pg"], writes=["lfT"])
        S.op("dve", lambda e: e.tensor_copy(iT[:], pg[:, 6:12]), reads=["pg"], writes=["iT"])
        S.op("pe", lambda e: e.matmul(pg[:, 12:18], triu[:], lfT[:], start=True, stop=True), reads=["lfT", "triu"], writes=["pg"])
        S.op("pe", lambda e: e.matmul(pg[:, 18:24], ones[:], lfT[:], start=True, stop=True), reads=["lfT", "ones"], writes=["pg"])
        S.op("pe", lambda e: e.matmul(pbrow[0:6, 0:128], lfT[:], triu[:], start=True, stop=True), reads=["lfT", "triu"], writes=["pbrow"])
        S.op("dve", lambda e: e.tensor_tensor(tmp6[:], iT[:], pg[:, 12:18], ALU.add), reads=["iT", "pg"], writes=["tmp6"])
        S.op("act", lambda e: e.activation(Ecol, tmp6[:], AF.Exp, bias=-LN8, scale=1.0), reads=["tmp6"], writes=["Ecol"])
        S.op("act", lambda e: e.activation(ebl, pg[:, 18:24], AF.Exp, scale=-1.0), reads=["pg"], writes=["ebl"])
        S.op("dve", lambda e: e.tensor_tensor(wcol[:], Ecol, ebl, ALU.mult), reads=["Ecol", "ebl"], writes=["wcol"])
        S.op("act", lambda e: e.activation(ebrA[:, c, :], pbrow[0:6, 0:128], AF.Exp), reads=["pbrow"], writes=["ebr"])
        for p in range(3):
            S.op("pe", lambda e, p=p: e.transpose(pT[:, p * 128:(p + 1) * 128], qk[:, 3 + p, tsl], identb[:]), reads=["qk", "identb"], writes=["pT"])
        for h in range(6):
            S.op("dve", lambda e, h=h: e.tensor_scalar(wkA[:, c, h // 2, (h % 2) * 64:(h % 2) * 64 + 64], pT[:, h * 64:(h + 1) * 64], wcol[:, h:h + 1], None, ALU.mult),
                 reads=["pT", "wcol"], writes=["wk"])

    def state_update(c, h):
        p = h // 2
        rs = slice((h % 2) * 64, (h % 2) * 64 + 64)
        S.op("pe", lambda e: e.matmul(pC[:, 0:256], wkA[:, c, p, :], Va[:, c, h, :], start=True, stop=True), reads=["wk", "Va0", "Va1"], writes=["pC"])
        S.op("dve", lambda e: e.scalar_tensor_tensor(Sst[rs, p, :], Sst[rs, p, :], eblA[rs, c, h:h + 1], pC[rs, 0:256], ALU.mult, ALU.add),
             reads=["Sst", "ebl", "pC", "Sbf"], writes=["Sst"])

    def outputs(c, h):
        tsl = slice(c * 128, (c + 1) * 128)
        p = h // 2
        rs = slice((h % 2) * 64, (h % 2) * 64 + 64)
        S.op("pe", lambda e: e.matmul(pS[:, 0:128], qk[rs, 3 + p, tsl], qk[rs, p, tsl], start=True, stop=True), reads=["qk"], writes=["pS"])
        S.op("dve", lambda e: e.scalar_tensor_tensor(Sw[:], pS[:, 0:128], EcolA[:, c, h:h + 1], mask01[:], ALU.mult, ALU.mult), reads=["pS", "Ecol", "mask01"], writes=["Sw"])
        S.op("pe", lambda e: e.matmul(pN[:, 0:128], Va[:, c, h, 0:128], Sw[:], start=True, stop=False), reads=["Va0", "Sw"], writes=["pN"])
        S.op("pe", lambda e: e.matmul(pN[:, 0:128], Sbf[rs, p, 0:128], qk[rs, p, tsl], start=False, stop=True), reads=["Sbf", "qk"], writes=["pN"])
        S.op("pe", lambda e: e.matmul(pD[:, 0:128], Va[:, c, h, 128:256], Sw[:], start=True, stop=False), reads=["Va1", "Sw"], writes=["pD"])
        S.op("pe", lambda e: e.matmul(pD[:, 0:128], Sbf[rs, p, 128:256], qk[rs, p, tsl], start=False, stop=True), reads=["Sbf", "qk"], writes=["pD"])
        S.op("pe", lambda e: e.matmul(pE[:, 0:128], selh[:, h, :], ebrA[:, c, :], start=True, stop=True), reads=["selh", "ebr"], writes=["pE"])
        S.op("act", lambda e: e.activation(aden[:], pD[:, 0:128], AF.Abs), reads=["pD"], writes=["aden"])
        S.op("dve", lambda e: e.tensor_tensor(aden[:], aden[:], pE[:, 0:128], ALU.max), reads=["aden", "pE"], writes=["aden"])
        S.op("dve", lambda e: e.reciprocal(aden[:], aden[:]), reads=["aden"], writes=["aden"])
        S.op("dve", lambda e: e.tensor_tensor(hm[:], pN[:, 0:128], aden[:], ALU.mult), reads=["pN", "aden"], writes=["hm"])
        m_ = mo[moi[0] % 2]; mk = "mo%d" % (moi[0] % 2); moi[0] += 1
        S.op("pool", lambda e: e.tensor_tensor(m_[:], hm[:], so[:, h, tsl], ALU.mult), reads=["hm", "so"], writes=[mk])
        S.dma(lambda e: e.dma_start(out=X["mlout"][h * 128:(h + 1) * 128, tsl], in_=m_[:]), reads=[mk], writes=["mlout"])

    S.op("pool", lambda e: e.memset(Sst[:], 0.0), writes=["Sst"])
    S.op("pool", lambda e: e.memset(blob[:, 768:772], 1.0), writes=["blobP"])
    for c in range(17):
        chunk_gates(c)
        if c == 1:
            S.op("act", lambda e: e.activation(Mst[:], Sst[:], AF.Copy), reads=["Sst"], writes=["Mst"])
            S.op("pool", lambda e: e.memset(Sst[:], 0.0), reads=["Mst"], writes=["Sst"])
        for h in range(6):
            state_update(c, h)
            if c >= 1:
                p = h // 2
                rs = slice((h % 2) * 64, (h % 2) * 64 + 64)
                S.op("pool", lambda e, p=p, rs=rs, h=h, c=c: e.tensor_tensor(blob[rs, 768 + p:769 + p], blob[rs, 768 + p:769 + p], eblA[rs, c, h:h + 1], ALU.mult),
                     reads=["ebl", "blobP"], writes=["blobP"])
    S.op("act", lambda e: e.activation(blob[:, 0:768], Sst[:].rearrange("p a b -> p (a b)"), AF.Copy), reads=["Sst"], writes=["blobS"])
    S.dma(lambda e: e.dma_start(out=X["stblob"][:, :], in_=blob[:]), reads=["blobS", "blobP"], writes=["stblob"])
    S.cc(lambda e: e.collective_compute("AllGather", ALU.bypass, replica_groups=GROUPS, ins=[X["stblob"][:, :].opt()], outs=[X["gS"][:, :].opt()]),
         reads=["stblob"], writes=["gS"])
    S.dma(lambda e: e.dma_start(out=G[:], in_=X["gS"].rearrange("(r p) x -> p r x", p=128)), reads=["gS"], writes=["G"])
    S.op("dve", lambda e: e.tensor_scalar(Sin[:], Mst[:], selq[:, 0:1], None, ALU.mult), reads=["Mst", "selq"], writes=["Sin"])
    for r in range(3):
        for p in range(3):
            S.op("dve", lambda e, r=r, p=p: e.scalar_tensor_tensor(Mst[:, p, :], Mst[:, p, :], G[:, r, 768 + p:769 + p], G[:, r, p * 256:(p + 1) * 256], ALU.mult, ALU.add),
                 reads=["Mst", "G"], writes=["Mst"])
        S.op("dve", lambda e, r=r: e.scalar_tensor_tensor(Sin[:], Mst[:], selq[:, r + 1:r + 2], Sin[:], ALU.mult, ALU.add), reads=["Mst", "selq", "Sin"], writes=["Sin"])
    S.op("pool", lambda e: e.memset(Sst[:], 0.0), reads=["blobS"], writes=["Sst"])
    S.op("pool", lambda e: e.memset(Sbf[:], 0.0), writes=["Sbf"])
    for c in range(17):
        if c == 1:
            S.op("act", lambda e: e.activation(Sst[:], Sin[:], AF.Copy), reads=["Sin", "Sst"], writes=["Sst"])
            S.op("act", lambda e: e.activation(Sbf[:], Sst[:], AF.Copy), reads=["Sst"], writes=["Sbf"])
        for h in range(6):
            outputs(c, h)
            state_update(c, h)
        S.op("act", lambda e: e.activation(Sbf[:], Sst[:], AF.Copy), reads=["Sst"], writes=["Sbf"])

    C.close()

    C = Ctx(nc, S)
    pC = C.ps("pC")
    hb_ = C.sb("hb", [128, 82], F32)
    S.dma(lambda e: e.dma_start(out=hb_[:], in_=X["hsel"][:, :]), writes=["hb"])
    PW = 16 + 128 + 16 + 2048
    px = C.sb("px", [128, PW], F32); pa = C.sb("pa", [128, PW], F32); pb_ = C.sb("pb", [128, PW], F32)
    py = C.sb("py", [128, TC], BF16); pwb = C.sb("pwb", [128, 4, 128], BF16); psc = C.sb("psc", [128, 4], F32); icn = C.sb("icn", [128, 4, 128], F32)
    S.dma(lambda e: e.dma_start(out=pwb[:], in_=pool_w_l.rearrange("g c d -> c g d")), writes=["pwb"], queue="pool")
    S.dma(lambda e: e.dma_start(out=psc[:], in_=pscale_l), writes=["psc"])
    S.dma(lambda e: e.dma_start(out=icn[:], in_=Cst["invcnt"][:, :, :]), writes=["icn"])
    post = [C.sb("post%d" % i, [128, 512], BF16) for i in range(2)]
    pi = 0
    for g in range(4):
        gs_ = slice(g * 128, (g + 1) * 128)
        S.op("pool", lambda e: e.memset(px[:, 0:16], 0.0), writes=["px"])
        S.dma(lambda e, gs_=gs_: e.dma_start(out=px[:, 16:144], in_=X["poolu"][gs_, 0:128]), writes=["px"])
        S.op("pool", lambda e, g=g: e.tensor_copy(px[:, 144:160], hb_[:, 18 + g * 16:18 + (g + 1) * 16]), reads=["hb"], writes=["px"])
        S.dma(lambda e, gs_=gs_: e.dma_start(out=px[:, 160:PW], in_=X["poolu"][gs_, 128:TC]), writes=["px"])
        S.op("pool", lambda e: e.tensor_tensor(pa[:, 1:PW], px[:, 1:PW], px[:, 0:PW - 1], ALU.add), reads=["px"], writes=["pa"])
        src, sk = pa, "pa"
        if g >= 1:
            S.op("pool", lambda e: e.tensor_tensor(pb_[:, 3:PW], pa[:, 3:PW], pa[:, 1:PW - 2], ALU.add), reads=["pa"], writes=["pb"])
            src, sk = pb_, "pb"
        if g >= 2:
            S.op("pool", lambda e: e.tensor_tensor(pa[:, 7:PW], pb_[:, 7:PW], pb_[:, 3:PW - 4], ALU.add), reads=["pb", "pa"], writes=["pa"])
            src, sk = pa, "pa"
        if g >= 3:
            S.op("pool", lambda e: e.tensor_tensor(pb_[:, 15:PW], pa[:, 15:PW], pa[:, 7:PW - 8], ALU.add), reads=["pa", "pb"], writes=["pb"])
            src, sk = pb_, "pb"
        w = float(2 ** (g + 1))
        S.op("dve", lambda e, g=g, src=src: e.tensor_tensor(src[:, 16:144], src[:, 16:144], icn[:, g, :], ALU.mult), reads=[sk, "icn"], writes=[sk])
        S.op("dve", lambda e, src=src: e.tensor_tensor(py[:, 0:128], src[:, 16:144], px[:, 16:144], ALU.subtract), reads=[sk, "px"], writes=["py"])
        S.op("dve", lambda e, src=src, w=w: e.scalar_tensor_tensor(py[:, 128:TC], src[:, 160:PW], 1.0 / w, px[:, 160:PW], ALU.mult, ALU.subtract),
             reads=[sk, "px"], writes=["py"])
        for (t0, tn) in TT:
            S.op("pe", lambda e, g=g, t0=t0, tn=tn: e.matmul(pC[:, 0:tn], pwb[:, g, :], py[:, t0:t0 + tn], start=True, stop=True), reads=["pwb", "py"], writes=["pC"])
            po = post[pi % 2]; pk = "post%d" % (pi % 2); pi += 1
            S.op("act", lambda e, po=po, g=g, tn=tn: e.activation(po[:, 0:tn], pC[:, 0:tn], AF.Copy, scale=psc[:, g:g + 1]), reads=["pC", "psc"], writes=[pk])
            S.dma(lambda e, po=po, g=g, t0=t0, tn=tn: e.dma_start(out=X["poolout"][g * 128:(g + 1) * 128, t0:t0 + tn], in_=po[:, 0:tn]), reads=[pk], writes=["poolout"])
    C.close()


def phase_unshuffle(nc, S, X, Cst):
    for h in range(6):
        S.cc(lambda e, h=h: e.collective_compute("AllGather", ALU.bypass, replica_groups=GROUPS, ins=[X["sbo_c"][h * 128:(h + 1) * 128, :].opt()], outs=[X["gO"][h].opt()]),
             writes=["gO"])
    S.barrier()
    S.emit()
    C = Ctx(nc, S)
    selq = C.sb("selq", [128, 4], F32)
    S.dma(lambda e: e.dma_start(out=selq[:], in_=Cst["selq"][:, :]), writes=["selq"])
    cd = [[C.sb("cd%d_%d" % (i, qq), [128, 16, 128], BF16) for qq in range(4)] for i in range(2)]
    acc = [C.sb("acc%d" % i, [128, 16, 128], BF16) for i in range(2)]
    S.dma(lambda e: e.dma_start(out=X["sbo_n"][:, 0:128], in_=X["sbo_c"][:, 0:128]), writes=["sbo_n0"])
    for h in range(6):
        i = h % 2
        for qq in range(4):
            dst = cd[i][qq][:].rearrange("p (jh jl) t -> p jh jl t", jl=4)
            for jl in range(4):
                S.dma(lambda e, dst=dst, h=h, qq=qq, jl=jl: e.dma_start(
                    out=dst[:, :, jl, :], in_=X["gO"][h, jl * 128:(jl + 1) * 128, (1 + 4 * qq) * 128:(5 + 4 * qq) * 128].rearrange("p (jh t) -> p jh t", t=128)),
                    writes=["cd%d_%d" % (i, qq)])
        a_ = acc[i]
        S.op("dve", lambda e, a_=a_, i=i: e.tensor_scalar(a_[:], cd[i][0][:], selq[:, 0:1], None, ALU.mult), reads=["cd%d_0" % i, "selq"], writes=["acc%d" % i])
        for qq in range(1, 4):
            S.op("dve", lambda e, a_=a_, i=i, qq=qq: e.scalar_tensor_tensor(a_[:], cd[i][qq][:], selq[:, qq:qq + 1], a_[:], ALU.mult, ALU.add),
                 reads=["cd%d_%d" % (i, qq), "selq", "acc%d" % i], writes=["acc%d" % i])
        S.dma(lambda e, a_=a_, h=h: e.dma_start(out=X["sbo_n"][h * 128:(h + 1) * 128, 128:TC], in_=a_[:].rearrange("p j t -> p (j t)")), reads=["acc%d" % i], writes=["sbo_n"])
    C.close()


def phase_precast(nc, S, X, w_out_l, w_gu_l, w_dn_l):
    wov = w_out_l.rearrange("(k p) d -> p k d", p=128)
    wgv = w_gu_l.rearrange("(k p) f -> p k f", p=128)
    wdv = w_dn_l.rearrange("(f p) d -> p f d", p=128)
    for st in range(22):
        S.dma(lambda e, st=st: e.dma_start(out=X["Wg"][st, 0], in_=wgv[:, :, st * 256:(st + 1) * 256]), writes=["Wg%d_0" % st], queue="pool")
        S.dma(lambda e, st=st: e.dma_start(out=X["Wg"][st, 1], in_=wgv[:, :, FF + st * 256:FF + (st + 1) * 256]), writes=["Wg%d_1" % st], queue="pool")
    for half in range(2):
        for dp in range(8):
            S.dma(lambda e, half=half, dp=dp: e.dma_start(out=X["Wd"][half, dp], in_=wdv[:, half * 22:(half + 1) * 22, dp * 256:(dp + 1) * 256]),
                  writes=["Wd%d_%d" % (half, dp)], queue="pool")
    for dp in range(8):
        S.dma(lambda e, dp=dp: e.dma_start(out=X["Wo"][dp], in_=wov[:, :, dp * 256:(dp + 1) * 256]), writes=["Wo%d" % dp], queue="pool")


def phase_p3(nc, S, X, hT, hN, w_out_l, gains_l, w_gu_l, w_dn_l):
    C = Ctx(nc, S)
    banks = [C.ps("bank%d" % i) for i in range(8)]
    ones_bf = C.sb("ones", [128, 128], BF16)
    S.op("pool", lambda e: e.memset(ones_bf[:], 1.0), writes=["ones"])
    gn = C.sb("gn", [128, 3, NK], F32)
    S.dma(lambda e: e.dma_start(out=gn[:], in_=gains_l), writes=["gn"])
    B1 = C.sb("B1", [128, NK, 512], BF16); B2 = C.sb("B2", [128, NK, 512], BF16)
    Y = C.sb("Y", [128, NK, 512], F32); H = C.sb("H", [128, NK, 512], F32)
    rr = C.sb("rr", [128, 512], F32); sl = C.sb("sl", [128, 512], F32)
    hid = C.sb("hid", [128, 22, 512], BF16)
    wo = [C.sb("wo%d" % i, [128, NK, 256], BF16) for i in range(2)]
    wg = [C.sb("wg%d" % i, [128, NK, 256], BF16) for i in range(2)]
    wu = [C.sb("wu%d" % i, [128, NK, 256], BF16) for i in range(2)]
    wd = [C.sb("wd%d" % i, [128, 22, 256], BF16) for i in range(2)]
    hv = hT.rearrange("(k p) t -> p k t", p=128)
    hnv = hN.rearrange("(k p) t -> p k t", p=128)
    wov = w_out_l.rearrange("(k p) d -> p k d", p=128)
    wgv = w_gu_l.rearrange("(k p) f -> p k f", p=128)
    wdv = w_dn_l.rearrange("(f p) d -> p f d", p=128)
    cnt = {"wo": 0, "wg": 0, "wd": 0, "bk": 0}

    def rstd(src, srck, sq, sqk, tn):
        emit_rstd(C, src, srck, sq, sqk, rr, ones_bf, banks[7], "bank7", tn)

    def do_tile(t0, tn):
        S.dma(lambda e: e.dma_start(out=H[:, :, 0:tn], in_=hv[:, :, t0:t0 + tn]), writes=["H"])
        S.dma(lambda e: e.dma_start(out=B1[:, 0:6, 0:tn], in_=X["mlout"].rearrange("(k p) t -> p k t", p=128)[:, :, t0:t0 + tn]), writes=["B1"])
        S.dma(lambda e: e.dma_start(out=B1[:, 6:12, 0:tn], in_=X["sbo_n"].rearrange("(k p) t -> p k t", p=128)[:, :, t0:t0 + tn]), writes=["B1"])
        S.dma(lambda e: e.dma_start(out=B1[:, 12:16, 0:tn], in_=X["poolout"].rearrange("(k p) t -> p k t", p=128)[:, :, t0:t0 + tn]), writes=["B1"])
        for dch in range(NK):
            if dch % 2 == 0:
                w = wo[cnt["wo"] % 2]; wk_ = "wo%d" % (cnt["wo"] % 2); cnt["wo"] += 1
                S.dma(lambda e, w=w, dch=dch: e.dma_start(out=w[:], in_=X["Wo"][dch // 2]), writes=[wk_])
            bk = cnt["bk"] % 4; cnt["bk"] += 1
            for k in range(NK):
                S.op("pe", lambda e, w=w, k=k, bk=bk, c0=(dch % 2) * 128: e.matmul(banks[bk][:, 0:tn], w[:, k, c0:c0 + 128], B1[:, k, 0:tn], start=(k == 0), stop=(k == NK - 1)),
                     reads=[wk_, "B1"], writes=["bank%d" % bk])
            S.op("dve", lambda e, dch=dch, bk=bk: e.tensor_copy(Y[:, dch, 0:tn], banks[bk][:, 0:tn]), reads=["bank%d" % bk], writes=["Y"])
        rstd(Y, "Y", B2, "B2", tn)
        for k in range(NK):
            S.op("dve", lambda e, k=k: e.scalar_tensor_tensor(Y[:, k, 0:tn], Y[:, k, 0:tn], gn[:, 0, k:k + 1], rr[:, 0:tn], ALU.mult, ALU.mult), reads=["Y", "gn", "rr"], writes=["Y"])
        S.op("pool", lambda e: e.tensor_tensor(H[:, :, 0:tn], H[:, :, 0:tn], Y[:, :, 0:tn], ALU.add), reads=["H", "Y"], writes=["H"])
        rstd(H, "H", B1, "B1", tn)
        for k in range(NK):
            S.op("dve", lambda e, k=k: e.scalar_tensor_tensor(B2[:, k, 0:tn], H[:, k, 0:tn], gn[:, 1, k:k + 1], rr[:, 0:tn], ALU.mult, ALU.mult), reads=["H", "gn", "rr"], writes=["B2"])
        for half in range(2):
            for fp in range(11):
                f0 = half * 22 + fp * 2
                g_ = wg[cnt["wg"] % 2]; u_ = wu[cnt["wg"] % 2]; gk = "wg%d" % (cnt["wg"] % 2); uk = "wu%d" % (cnt["wg"] % 2); cnt["wg"] += 1
                S.dma(lambda e, g_=g_, f0=f0: e.dma_start(out=g_[:], in_=X["Wg"][f0 // 2, 0]), writes=[gk])
                S.dma(lambda e, u_=u_, f0=f0: e.dma_start(out=u_[:], in_=X["Wg"][f0 // 2, 1]), writes=[uk], queue="act")
                for j in range(2):
                    bg = (cnt["bk"] % 2); cnt["bk"] += 1
                    pg_, pu_ = banks[bg], banks[2 + bg]
                    for k in range(NK):
                        S.op("pe", lambda e, g_=g_, k=k, j=j, pg_=pg_: e.matmul(pg_[:, 0:tn], g_[:, k, j * 128:(j + 1) * 128], B2[:, k, 0:tn], start=(k == 0), stop=(k == NK - 1)),
                             reads=[gk, "B2"], writes=["bank%d" % bg])
                    for k in range(NK):
                        S.op("pe", lambda e, u_=u_, k=k, j=j, pu_=pu_: e.matmul(pu_[:, 0:tn], u_[:, k, j * 128:(j + 1) * 128], B2[:, k, 0:tn], start=(k == 0), stop=(k == NK - 1)),
                             reads=[uk, "B2"], writes=["bank%d" % (2 + bg)])
                    S.op("act", lambda e, pg_=pg_: e.activation(sl[:, 0:tn], pg_[:, 0:tn], AF.Silu), reads=["bank%d" % bg], writes=["sl"])
                    S.op("dve", lambda e, pu_=pu_, fi=fp * 2 + j: e.tensor_tensor(hid[:, fi, 0:tn], sl[:, 0:tn], pu_[:, 0:tn], ALU.mult),
                         reads=["sl", "bank%d" % (2 + bg)], writes=["hid"])
            for dch in range(NK):
                if dch % 2 == 0:
                    w = wd[cnt["wd"] % 2]; wk_ = "wd%d" % (cnt["wd"] % 2); cnt["wd"] += 1
                    S.dma(lambda e, w=w, dch=dch, half=half: e.dma_start(out=w[:], in_=X["Wd"][half, dch // 2]), writes=[wk_])
                bk = 4 + (cnt["bk"] % 3); cnt["bk"] += 1
                for fi in range(22):
                    S.op("pe", lambda e, w=w, fi=fi, bk=bk, c0=(dch % 2) * 128: e.matmul(banks[bk][:, 0:tn], w[:, fi, c0:c0 + 128], hid[:, fi, 0:tn], start=(fi == 0), stop=(fi == 21)),
                         reads=[wk_, "hid"], writes=["bank%d" % bk])
                if half == 0:
                    S.op("dve", lambda e, dch=dch, bk=bk: e.tensor_copy(Y[:, dch, 0:tn], banks[bk][:, 0:tn]), reads=["bank%d" % bk], writes=["Y"])
                else:
                    S.op("dve", lambda e, dch=dch, bk=bk: e.tensor_tensor(Y[:, dch, 0:tn], Y[:, dch, 0:tn], banks[bk][:, 0:tn], ALU.add), reads=["bank%d" % bk, "Y"], writes=["Y"])
        rstd(Y, "Y", B1, "B1", tn)
        for k in range(NK):
            S.op("dve", lambda e, k=k: e.scalar_tensor_tensor(Y[:, k, 0:tn], Y[:, k, 0:tn], gn[:, 2, k:k + 1], rr[:, 0:tn], ALU.mult, ALU.mult), reads=["Y", "gn", "rr"], writes=["Y"])
        S.op("pool", lambda e: e.tensor_tensor(H[:, :, 0:tn], H[:, :, 0:tn], Y[:, :, 0:tn], ALU.add), reads=["H", "Y"], writes=["H"])
        return S.dma(lambda e: e.dma_start(out=hnv[:, :, t0:t0 + tn], in_=H[:, :, 0:tn]), reads=["H"], writes=["hN"])
    toks = []
    for (t0_, tn_) in TT:
        toks.append(do_tile(t0_, tn_))
    C.close()
    return toks


def build_fused(NL):
    nc = bass.Bass("TRN2", target_bir_lowering=False)
    def din(name, shape, dt=F32):
        return nc.dram_tensor(name, list(shape), dt, kind="ExternalInput").ap()
    def dint(name, shape, dt=F32):
        return nc.dram_tensor(name, list(shape), dt).ap()
    hT0 = din("hT0", [D, TC])
    w_in = din("w_in", [NL, D, IN_W]); g_pre = din("g_pre", [NL, 128, NK]); convw = din("convw", [NL, 128, 6, 4]); gbias = din("gbias", [NL, 6, 2])
    pool_w = din("pool_w", [NL, 4, 128, 128]); pscale = din("pscale", [NL, 128, 4]); w_out = din("w_out", [NL, D, D]); gains = din("gains", [NL, 128, 3, NK])
    w_gu = din("w_gu", [NL, D, 2 * FF]); w_dn = din("w_dn", [NL, FF, D])
    Cst = {"mask4": din("mask4", [128, 4, 128], BF16), "maskm": din("maskm", [16, 128], BF16), "identb": din("identb", [128, 128], BF16),
           "ntri": din("ntri", [128, 128], BF16), "selq4": din("selq4", [128, 4]), "selq": din("selq", [128, 4]), "selprev": din("selprev", [128, 5]),
           "triu": din("triu", [128, 128]), "ones128": din("ones128", [128, 128]), "i6": din("i6", [6, 6]), "selh": din("selh", [6, 6, 128]),
           "mask01": din("mask01", [128, 128]), "invcnt": din("invcnt", [128, 4, 128])}
    hN = nc.dram_tensor("hN", [D, TC], F32, kind="ExternalOutput").ap()
    X = {"qkpre": dint("x_qkpre", [768, TC]), "mlo": dint("x_mlo", [768, TC], BF16), "gates": dint("x_gates", [12, TC]),
         "sbq": dint("x_sbq", [768, TC], BF16), "sbk": dint("x_sbk", [768, TC], BF16), "poolu": dint("x_poolu", [512, TC]),
         "mlv": dint("x_mlv", [6, TC, 128], BF16), "sbv": dint("x_sbv", [6, TC, 128], BF16), "tail": dint("x_tail", [128, 82]),
         "gK": dint("x_gK", [6, 512, TC], BF16), "gQ": dint("x_gQ", [6, 512, TC], BF16), "gV": dint("x_gV", [6, 4 * TC, 128], BF16), "gT": dint("x_gT", [512, 82]),
         "sbo_c": dint("x_sbo_c", [768, TC], BF16), "gO": dint("x_gO", [6, 512, TC], BF16), "sbo_n": dint("x_sbo_n", [768, TC], BF16),
         "mlout": dint("x_mlout", [768, TC], BF16), "poolout": dint("x_poolout", [512, TC], BF16),
         "stblob": dint("x_stblob", [128, 772]), "Wg": dint("x_Wg", [22, 2, 128, NK, 256], BF16), "Wd": dint("x_Wd", [2, 8, 128, 22, 256], BF16), "Wo": dint("x_Wo", [8, 128, NK, 256], BF16), "hsel": dint("x_hsel", [128, 82]), "gS": dint("x_gS", [512, 772])}
    hbuf = [dint("x_hA", [D, TC]), dint("x_hB", [D, TC])]
    with contextlib.ExitStack() as es:
        S = Sched(nc, es)
        toks = []
        for l in range(NL):
            h_in = hT0 if l == 0 else hbuf[(l - 1) % 2]
            h_out = hN if l == NL - 1 else hbuf[l % 2]
            phase_p1(nc, S, h_in, w_in[l], g_pre[l], X)
            phase_gather1(nc, S, X)
            phase_precast(nc, S, X, w_out[l], w_gu[l], w_dn[l])
            phase_attn(nc, S, X, Cst)
            phase_ml(nc, S, X, Cst, convw[l], gbias[l], pool_w[l], pscale[l])
            phase_unshuffle(nc, S, X, Cst)
            toks = phase_p3(nc, S, X, h_in, h_out, w_out[l], gains[l], w_gu[l], w_dn[l])
        S.barrier()
        S.emit()
    return nc

import ml_dtypes
_BF = ml_dtypes.bfloat16
_PROGS = {}


def _lay_g(g):
    return np.ascontiguousarray(np.asarray(g, np.float32).reshape(16, 128).T)


def _consts(q):
    j = np.arange(128)
    s = j[:, None]
    t = j[None, :]
    m4 = np.zeros((128, 4, 128), np.float32)
    for jj in range(4):
        if jj == q:
            m4[:, jj, :] = np.where(s < t, 0.0, -30000.0)
        elif jj > q:
            m4[:, jj, :] = -30000.0
    sm = np.arange(16)[:, None]
    mmeta = np.where((t >= 112) & (sm < t - 112), 0.0, -30000.0)
    ntri = np.where(s >= t, -1.0, 0.0)
    d = {"mask4": m4.astype(_BF), "maskm": mmeta.astype(_BF), "identb": np.eye(128).astype(_BF), "ntri": ntri.astype(_BF)}
    oh = np.zeros((128, 4), np.float32)
    oh[:, q] = 1.0
    d["selq"] = oh
    d["selq4"] = (oh * np.float32(QSCALE)).astype(np.float32)
    sp = np.zeros((128, 5), np.float32)
    if q == 0:
        sp[:, 4] = 1.0
    else:
        sp[:, q - 1] = 1.0
    d["selprev"] = sp
    d["triu"] = (s <= t).astype(np.float32)
    d["ones128"] = np.ones((128, 128), np.float32)
    d["i6"] = np.eye(6, dtype=np.float32)
    d["mask01"] = (s <= t).astype(np.float32)
    selh = np.zeros((6, 6, 128), np.float32)
    for hh in range(6):
        selh[hh, hh, :] = 1
    d["selh"] = selh
    ic = np.zeros((128, 4, 128), np.float32)
    for gg in range(4):
        w = 2 ** (gg + 1)
        ic[:, gg, :] = 1.0 / np.clip(j - 111, 1, w).astype(np.float32)
    d["invcnt"] = ic
    return d


def _shared_inputs(NL, w_in, ml_conv_w, ml_igate_b, ml_fgate_b, pool_w, pool_scale, w_out, g_mix_pre, g_mix_post, g_ffn_pre, g_ffn_post, w_gate_up, w_down):
    f = lambda a: np.ascontiguousarray(np.asarray(a, np.float32)[:NL])
    d = {"w_in": f(w_in), "w_out": f(w_out), "w_gu": f(w_gate_up), "w_dn": f(w_down), "pool_w": f(pool_w)}
    d["g_pre"] = np.stack([_lay_g(g_mix_pre[l]) for l in range(NL)])
    d["convw"] = np.stack([np.ascontiguousarray(np.asarray(ml_conv_w[l], np.float32).reshape(4, 6, 128).transpose(2, 1, 0)) for l in range(NL)])
    d["gbias"] = np.stack([np.stack([np.asarray(ml_igate_b[l], np.float32), np.asarray(ml_fgate_b[l], np.float32)], 1) for l in range(NL)])
    d["pscale"] = np.stack([np.ascontiguousarray(np.asarray(pool_scale[l], np.float32).T) for l in range(NL)])
    d["gains"] = np.stack([np.stack([_lay_g(g_mix_post[l]), _lay_g(g_ffn_pre[l]), _lay_g(g_ffn_post[l])], 1) for l in range(NL)])
    return {k: np.ascontiguousarray(v) for k, v in d.items()}


def run_fused(NL, x, meta_tokens, **params):
    x = np.asarray(x, np.float32)
    meta = np.asarray(meta_tokens, np.float32)
    if NL not in _PROGS:
        _PROGS[NL] = build_fused(NL)
    nc = _PROGS[NL]
    shared = _shared_inputs(NL, **params)
    ins = []
    for c in range(8):
        b, q = c // 4, c % 4
        h = np.zeros((TC, D), np.float32)
        h[112:128] = meta
        h[128:] = x[b, q * 2048:(q + 1) * 2048]
        d = dict(shared)
        d.update(_consts(q))
        d["hT0"] = np.ascontiguousarray(h.T)
        ins.append(d)
    res = run_bass_kernel_spmd(nc, ins, core_ids=list(range(8))).results
    out = np.zeros((2, 8192, D), np.float32)
    for c in range(8):
        b, q = c // 4, c % 4
        out[b, q * 2048:(q + 1) * 2048] = res[c]["hN"][:, 128:].T
    return out


def kernel(x, meta_tokens, w_in, ml_conv_w, ml_igate_b, ml_fgate_b, pool_w, pool_scale, w_out,
           g_mix_pre, g_mix_post, g_ffn_pre, g_ffn_post, w_gate_up, w_down):
    return run_fused(4, x, meta_tokens, w_in=w_in, ml_conv_w=ml_conv_w, ml_igate_b=ml_igate_b, ml_fgate_b=ml_fgate_b, pool_w=pool_w,
                     pool_scale=pool_scale, w_out=w_out, g_mix_pre=g_mix_pre, g_mix_post=g_mix_post, g_ffn_pre=g_ffn_pre,
                     g_ffn_post=g_ffn_post, w_gate_up=w_gate_up, w_down=w_down)
```
